# Optimizing a Trainium2 kernel written in Bass

```python
import jax, jax.numpy as jnp
from jax import lax
import numpy as np

D_MODEL = 4096
BATCH = 4
SEQ = 4096
DEPTH = 1

MIX_WIDTH = D_MODEL
M_HEADS = 4
M_DV = MIX_WIDTH // 2 // M_HEADS
M_DK = M_DV // 2
G_HEADS = 4
G_DV = MIX_WIDTH // 2 // G_HEADS
G_DK = G_DV // 2
G_RANK = 16
G_TAU = 16.0
QK_CONV = 4
FFN_CONV = 3
D_FF = ((8 * D_MODEL // 3) + 127) // 128 * 128
CHUNK = 64
EPS = 1e-6

IN_SPLITS = (
    M_HEADS * M_DK,
    M_HEADS * M_DK,
    M_HEADS * M_DV,
    M_HEADS * M_DV,
    M_HEADS,
    M_HEADS,
    G_HEADS * G_DK,
    G_HEADS * G_DK,
    G_HEADS * G_DV,
    G_HEADS * G_DV,
    G_RANK,
)
D_IN = sum(IN_SPLITS)

kernel_name = "hymba_mlstm_gla_convffn"


def rmsnorm(x, g):
    xf = x.astype(jnp.float32)
    xf = xf * lax.rsqrt(jnp.mean(xf * xf, axis=-1, keepdims=True) + EPS)
    return (xf * g.astype(jnp.float32)).astype(x.dtype)


def head_rmsnorm(h, g):
    B, S, H, Dv = h.shape
    h = h * lax.rsqrt(jnp.mean(h * h, axis=-1, keepdims=True) + EPS)
    return h.reshape(B, S, H * Dv) * g.astype(jnp.float32)


def causal_dwconv(x, w, b):
    K, C = w.shape
    y = lax.conv_general_dilated(
        x, w[:, None, :].astype(x.dtype), window_strides=(1,), padding=[(K - 1, 0)],
        dimension_numbers=("NWC", "WIO", "NWC"), feature_group_count=C)
    return y + b.astype(x.dtype)


def to_chunks(a):
    B, S, H = a.shape[:3]
    a = a.reshape(B, S // CHUNK, CHUNK, H, *a.shape[3:])
    return jnp.moveaxis(a, (1, 3), (0, 2))


def from_chunks(a):
    NC, B, H, L, D = a.shape
    return jnp.moveaxis(a, (0, 2), (1, 3)).reshape(B, NC * L, H, D)


def mlstm_chunkwise(q, k, v, i_pre, f_pre):
    f32 = jnp.float32
    B, S, H, DK = q.shape
    DV = v.shape[-1]
    qc = to_chunks(q.astype(f32) * (DK ** -0.5))
    kc = to_chunks(k.astype(f32))
    vc = to_chunks(v.astype(f32))
    ig = to_chunks(i_pre.astype(f32))
    bcum = jnp.cumsum(to_chunks(jax.nn.log_sigmoid(f_pre.astype(f32))), axis=-1)
    causal = jnp.tril(jnp.ones((CHUNK, CHUNK), dtype=bool))

    def step(carry, inp):
        C, n, m = carry
        q_, k_, v_, b, i_ = inp
        D = jnp.where(causal, b[..., :, None] - b[..., None, :] + i_[..., None, :], -jnp.inf)
        inter = b + m[..., None]
        m_t = jnp.maximum(inter, jnp.max(D, axis=-1))
        g = jnp.exp(inter - m_t)
        s_qk = jnp.einsum("bhtd,bhsd->bhts", q_, k_) * jnp.exp(D - m_t[..., None])
        num = g[..., None] * jnp.einsum("bhtd,bhde->bhte", q_, C) + jnp.einsum("bhts,bhse->bhte", s_qk, v_)
        den = g * jnp.einsum("bhtd,bhd->bht", q_, n) + jnp.sum(s_qk, axis=-1)
        h = num / jnp.maximum(jnp.abs(den), jnp.exp(-m_t))[..., None]
        bL = b[..., -1]
        a_s = bL[..., None] - b + i_
        m_new = jnp.maximum(bL + m, jnp.max(a_s, axis=-1))
        ws = jnp.exp(a_s - m_new[..., None])
        gC = jnp.exp(bL + m - m_new)
        C = gC[..., None, None] * C + jnp.einsum("bhs,bhsd,bhse->bhde", ws, k_, v_)
        n = gC[..., None] * n + jnp.einsum("bhs,bhsd->bhd", ws, k_)
        return (C, n, m_new), h

    init = (jnp.zeros((B, H, DK, DV), f32), jnp.zeros((B, H, DK), f32), jnp.zeros((B, H), f32))
    _, h = lax.scan(step, init, (qc, kc, vc, bcum, ig))
    return from_chunks(h)


def gla_chunkwise(q, k, v, log_a):
    f32 = jnp.float32
    B, S, H, DK = q.shape
    DV = v.shape[-1]
    qc = to_chunks(q.astype(f32) * (DK ** -0.5))
    kc = to_chunks(k.astype(f32))
    vc = to_chunks(v.astype(f32))
    bcum = jnp.cumsum(to_chunks(log_a.astype(f32)), axis=3)
    causal = jnp.tril(jnp.ones((CHUNK, CHUNK), dtype=bool))[..., None]

    def step(Sst, inp):
        q_, k_, v_, b = inp
        inter = jnp.einsum("bhtd,bhde->bhte", q_ * jnp.exp(b), Sst)
        diff = b[..., :, None, :] - b[..., None, :, :]
        decay = jnp.exp(jnp.where(causal, diff, -jnp.inf))
        A = jnp.einsum("bhtd,bhsd,bhtsd->bhts", q_, k_, decay)
        intra = jnp.einsum("bhts,bhse->bhte", A, v_)
        bL = b[..., -1, :]
        kdec = k_ * jnp.exp(bL[..., None, :] - b)
        Sst = jnp.exp(bL)[..., None] * Sst + jnp.einsum("bhsd,bhse->bhde", kdec, v_)
        return Sst, inter + intra

    _, o = lax.scan(step, jnp.zeros((B, H, DK, DV), f32), (qc, kc, vc, bcum))
    return from_chunks(o)


def setup_inputs(seed: int = 0) -> dict:
    key = jax.random.key(seed)
    ks = jax.random.split(key, 24)
    f32 = jnp.float32
    nrm = lambda k, shape, s: jax.random.normal(k, shape, f32) * s
    return {
        "x": nrm(ks[0], (BATCH, SEQ, D_MODEL), 1.0),
        "ln1_g": 1.0 + nrm(ks[1], (D_MODEL,), 0.02),
        "w_in": nrm(ks[2], (D_MODEL, D_IN), D_MODEL ** -0.5),
        "mlstm_conv_w": nrm(ks[3], (QK_CONV, 2 * M_HEADS * M_DK), QK_CONV ** -0.5),
        "mlstm_conv_b": nrm(ks[4], (2 * M_HEADS * M_DK,), 0.01),
        "mlstm_i_b": nrm(ks[5], (M_HEADS,), 0.1),
        "mlstm_f_b": jnp.linspace(3.0, 6.0, M_HEADS, dtype=f32) + nrm(ks[6], (M_HEADS,), 0.1),
        "mlstm_norm_g": 1.0 + nrm(ks[7], (M_HEADS * M_DV,), 0.02),
        "gla_a_w2": nrm(ks[8], (G_RANK, G_HEADS * G_DK), G_RANK ** -0.5),
        "gla_a_b": nrm(ks[9], (G_HEADS * G_DK,), 0.01),
        "gla_norm_g": 1.0 + nrm(ks[10], (G_HEADS * G_DV,), 0.02),
        "w_out": nrm(ks[11], (MIX_WIDTH, D_MODEL), MIX_WIDTH ** -0.5),
        "ln2_g": 1.0 + nrm(ks[12], (D_MODEL,), 0.02),
        "w_ffn_gate": nrm(ks[13], (D_MODEL, D_FF), D_MODEL ** -0.5),
        "w_ffn_up": nrm(ks[14], (D_MODEL, D_FF), D_MODEL ** -0.5),
        "ffn_conv_w": nrm(ks[15], (FFN_CONV, D_FF), FFN_CONV ** -0.5),
        "ffn_conv_b": nrm(ks[16], (D_FF,), 0.01),
        "w_ffn_down": nrm(ks[17], (D_FF, D_MODEL), D_FF ** -0.5),
        "lnf_g": 1.0 + nrm(ks[18], (D_MODEL,), 0.02),
    }


def reference(x, ln1_g, w_in, mlstm_conv_w, mlstm_conv_b, mlstm_i_b, mlstm_f_b, mlstm_norm_g,
              gla_a_w2, gla_a_b, gla_norm_g, w_out, ln2_g, w_ffn_gate, w_ffn_up,
              ffn_conv_w, ffn_conv_b, w_ffn_down, lnf_g):
    B, S, _ = x.shape
    split_idx = np.cumsum(IN_SPLITS)[:-1].tolist()
    for _layer in range(DEPTH):
        h = rmsnorm(x, ln1_g)
        proj = h @ w_in
        mq, mk, mv, mo, mi, mf, gq, gk, gv, gg, ga = jnp.split(proj, split_idx, axis=-1)

        mqk = jax.nn.silu(causal_dwconv(jnp.concatenate([mq, mk], axis=-1), mlstm_conv_w, mlstm_conv_b))
        mq, mk = jnp.split(mqk, 2, axis=-1)
        hm = mlstm_chunkwise(mq.reshape(B, S, M_HEADS, M_DK), mk.reshape(B, S, M_HEADS, M_DK),
                             mv.reshape(B, S, M_HEADS, M_DV), mi + mlstm_i_b, mf + mlstm_f_b)
        ym = head_rmsnorm(hm, mlstm_norm_g) * jax.nn.sigmoid(mo.astype(jnp.float32))

        log_a = jax.nn.log_sigmoid((ga @ gla_a_w2 + gla_a_b).astype(jnp.float32)) / G_TAU
        hg = gla_chunkwise(gq.reshape(B, S, G_HEADS, G_DK), gk.reshape(B, S, G_HEADS, G_DK),
                           gv.reshape(B, S, G_HEADS, G_DV), log_a.reshape(B, S, G_HEADS, G_DK))
        yg = head_rmsnorm(hg, gla_norm_g) * jax.nn.silu(gg.astype(jnp.float32))

        mix = jnp.concatenate([ym, yg], axis=-1).astype(x.dtype)
        x = x + mix @ w_out

        h2 = rmsnorm(x, ln2_g)
        gate = causal_dwconv(h2 @ w_ffn_gate, ffn_conv_w, ffn_conv_b)
        x = x + (jax.nn.silu(gate) * (h2 @ w_ffn_up)) @ w_ffn_down
    return rmsnorm(x, lnf_g)
```

```python
import contextlib
import numpy as np
import concourse.bass as bass
import concourse.mybir as mybir
from concourse.bass_utils import run_bass_kernel_spmd

F32 = mybir.dt.float32
BF16 = mybir.dt.bfloat16
ACT = mybir.ActivationFunctionType
ALU = mybir.AluOpType

P = 128
D = 4096
KD = 32
S = 4096
DFF = 11008
NF = 86
EPS = 1e-6
SH = 2176
RING = 8
import os
KSTOP = int(os.environ.get("KSTOP", "0"))
KDBG = int(os.environ.get("KDBG", "0"))


class StopEmit(Exception):
    pass


def ck(n):
    if KSTOP == n:
        raise StopEmit()


class Tracker:
    def __init__(self):
        self.lists = {k: [] for k in ("pe", "act", "dve", "pool", "sync")}
        self.cnt = {k: 0 for k in ("pe", "act", "dve", "pool")}
        self.lastw = {}
        self.readers = {}
        self.waited = {}
        self.ring_idx = {"sync": 0, "pool": 0}
        self.final = {}

    def _deps(self, reads, writes):
        d = {}
        for r in reads:
            t = self.lastw.get(r)
            if t:
                d[t[0]] = max(d.get(t[0], 0), t[1])
        for w in writes:
            t = self.lastw.get(w)
            if t:
                d[t[0]] = max(d.get(t[0], 0), t[1])
            for k, v in self.readers.get(w, {}).items():
                d[k] = max(d.get(k, 0), v)
        return d

    def _record(self, tok, reads, writes):
        for w in writes:
            self.lastw[w] = tok
            self.readers[w] = {}
        for r in reads:
            rd = self.readers.setdefault(r, {})
            rd[tok[0]] = max(rd.get(tok[0], 0), tok[1])
        self.final[tok[0]] = max(self.final.get(tok[0], 0), tok[1])

    def _emit(self, lst, deps, fn, inc):
        waits = []
        for k, v in deps.items():
            if self.waited.get((lst, k), 0) >= v:
                continue
            self.waited[(lst, k)] = v
            waits.append((k, v))
        self.lists[lst].append((waits, fn, inc))

    def op(self, eng, fn, reads=(), writes=()):
        writes = list(writes) + [r for r in reads if isinstance(r, tuple) and r[0] == "ps"]
        deps = self._deps(reads, writes)
        if eng == "pe":
            deps.pop("pe", None)
        self.cnt[eng] += 1
        self._emit(eng, deps, fn, (eng, 1))
        self._record((eng, self.cnt[eng]), reads, writes)

    def dma(self, q, out, in_, reads=(), writes=()):
        deps = self._deps(reads, writes)
        j = self.ring_idx[q]
        self.ring_idx[q] += 1
        key = (q + "ring", j % RING)
        if j >= RING:
            deps[key] = max(deps.get(key, 0), 16 * (j // RING))
        self._emit(q, deps, lambda e: e.dma_start(out=out, in_=in_), (key, 16))
        self._record((key, 16 * (j // RING + 1)), reads, writes)

    def custom(self, lst, fn, tok, reads=(), writes=()):
        deps = self._deps(reads, writes)
        self._emit(lst, deps, fn, (tok[0], 1))
        self._record(tok, reads, writes)

    def barrier(self):
        for lst in self.lists:
            self._emit(lst, dict(self.final), None, None)


def build_nc():
    nc = bass.Bass("TRN2", target_bir_lowering=False)
    T = Tracker()

    def din(name, shape, dt=F32):
        if os.environ.get("KSMALL") and name in ("x1", "x2", "wfm", "wtm", "wo", "wg", "wu", "wd"):
            shape = [128, 128]
        return nc.dram_tensor(name, list(shape), dt, kind="ExternalInput").ap()

    x1 = din("x1", [S, D]); x2 = din("x2", [SH, D])
    wfm = din("wfm", [D, 2048]); wtm = din("wtm", [D, 4096]); wsm = din("wsm", [D, 20])
    wo = din("wo", [D, D]); wg = din("wg", [D, DFF]); wu = din("wu", [D, DFF]); wd = din("wd", [DFF, D])
    g1t_d = din("g1t", [P, KD]); g2t_d = din("g2t", [P, KD]); g3_d = din("g3", [1, D]); gn_d = din("gn", [1, 2048])
    cwb_d = din("cwb", [P, 8, 5]); gb_d = din("gb", [P, 4]); aw2_d = din("aw2", [16, 512]); ab_d = din("ab", [P, 4])
    fcw_d = din("fcw", [P, NF, 4]); sel_d = din("sel", [P, 2])
    ident_d = din("ident", [P, P]); tri_d = din("tri", [P, P]); segm_d = din("segm", [P, 512])
    out = nc.dram_tensor("out", [2048, D], F32, kind="ExternalOutput").ap()
    send = nc.dram_tensor("send", [2 * SH, 2048], BF16).ap()
    gath = nc.dram_tensor("gath", [17 * 512, 2048], BF16).ap()
    if KDBG:
        dbg_send = nc.dram_tensor("dbg_send", [2 * SH, 2048], BF16, kind="ExternalOutput").ap()
        dbg_x1 = nc.dram_tensor("dbg_x1", [2048, D], F32, kind="ExternalOutput").ap()

    es = contextlib.ExitStack()

    def sb(name, shape, dt=F32):
        return es.enter_context(nc.sbuf_tensor("sb_" + name, list(shape), dt))

    sems = {}
    for k in ("pe", "act", "dve", "pool", "cc"):
        sems[k] = es.enter_context(nc.semaphore("s_" + k))
    for q in ("sync", "pool"):
        for r in range(RING):
            sems[(q + "ring", r)] = es.enter_context(nc.semaphore(f"r_{q}{r}"))
    ps = [es.enter_context(nc.psum_tensor(f"ps{b}", [P, 512], F32)) for b in range(8)]

    ident_f = sb("ident_f", [P, P]); ident_b = sb("ident_b", [P, P], BF16)
    tri = sb("tri", [P, P]); segm = sb("segm", [P, 512])
    ones_f = sb("ones_f", [P, P]); ones_b = sb("ones_b", [P, 2], BF16)
    g1t = sb("g1t", [P, KD]); g2t = sb("g2t", [P, KD])
    cwb = sb("cwb", [P, 8, 5]); gb = sb("gb", [P, 4]); ngb = sb("ngb", [P, 4])
    ab = sb("ab", [P, 4]); nab = sb("nab", [P, 4])
    fcw = sb("fcw", [P, NF, 4]); sel = sb("sel", [P, 2])
    aw2 = sb("aw2", [16, 512], BF16)
    wsm_sb = sb("wsm_sb", [P, KD, 20], BF16)
    slabs = [sb(f"slab{i}", [P, 2048], BF16) for i in range(3)]
    sm = sb("sm", [P, 64])
    slab_i = [0]

    def next_slab():
        i = slab_i[0] % len(slabs)
        slab_i[0] += 1
        return i

    for dst, src, nm in ((ident_f, ident_d, "ident_f"), (tri, tri_d, "tri"), (segm, segm_d, "segm"),
                         (g1t, g1t_d, "g1t"), (g2t, g2t_d, "g2t"), (gb, gb_d, "gb"), (ab, ab_d, "ab"),
                         (sel, sel_d, "sel")):
        T.dma("sync", dst[:], src, writes=[nm])
    T.dma("sync", cwb[:], cwb_d, writes=["cwb"])
    T.dma("sync", fcw[:], fcw_d, writes=["fcw"])
    T.dma("pool", aw2[:], aw2_d, writes=["aw2"])
    T.dma("pool", wsm_sb[:], wsm.rearrange("(k p) c -> p k c", p=P), writes=["wsm"])
    T.op("pool", lambda e: e.memset(ones_f[:], 1.0), writes=["ones_f"])
    T.op("pool", lambda e: e.memset(ones_b[:], 1.0), writes=["ones_b"])
    T.op("dve", lambda e: e.tensor_copy(out=ident_b[:], in_=ident_f[:]), reads=["ident_f"], writes=["ident_b"])
    T.op("dve", lambda e: e.tensor_scalar(out=ngb[:], in0=gb[:], scalar1=-1.0, scalar2=None, op0=ALU.mult),
         reads=["gb"], writes=["ngb"])
    T.op("dve", lambda e: e.tensor_scalar(out=nab[:], in0=ab[:], scalar1=-1.0, scalar2=None, op0=ALU.mult),
         reads=["ab"], writes=["nab"])

    AT = sb("AT", [P, KD, 512], BF16)
    ATr = [("AT", t) for t in range(4)]

    def rstd_from_ssq(ssq_ap, dst_ap, n, key):
        T.op("dve", lambda e: e.tensor_scalar(out=dst_ap, in0=ssq_ap, scalar1=1.0 / n, scalar2=EPS,
                                              op0=ALU.mult, op1=ALU.add), reads=[key], writes=[key])
        T.op("act", lambda e: e.activation(out=dst_ap, in_=dst_ap, func=ACT.Sqrt), reads=[key], writes=[key])
        T.op("dve", lambda e: e.reciprocal(out=dst_ap, in_=dst_ap), reads=[key], writes=[key])

    p1 = contextlib.ExitStack()

    def sb1(name, shape, dt=F32):
        return p1.enter_context(nc.sbuf_tensor("sb_" + name, list(shape), dt))

    xb = sb1("xb", [P, D]); junk = sb1("junk", [P, 512], BF16)
    pre = sb1("pre", [P, 8, 515]); qk = sb1("qk", [P, 8, 512], BF16); gqk = sb1("gqk", [P, 8, 512], BF16)
    tmpc = sb1("tmpc", [P, 512]); tmpc2 = sb1("tmpc2", [P, 512]); tmpcs = [tmpc, tmpc2]
    V = sb1("V", [P, 4, 2048], BF16); OG = sb1("OG", [P, 4, 2048], BF16); GN = sb1("GN", [P, 2048])
    gaT = sb1("gaT", [16, 512], BF16)
    EQg = sb1("EQg", [P, 4, 512]); EKg = sb1("EKg", [P, 4, 512]); spt = sb1("spt", [P, 512]); csp = sb1("csp", [P, 512])
    EQm = sb1("EQm", [P, 2, P]); EKm = sb1("EKm", [P, 2, P]); rb = sb1("rb", [P, P]); ru = sb1("ru", [P, P])
    S32 = sb1("S32", [P, 8, 512]); Sbf = sb1("Sbf", [P, 8, 512], BF16)
    n32 = sb1("n32", [P, 4, 2]); nbf = sb1("nbf", [P, 4, 2], BF16)
    qs = [sb1(f"qs{i}", [P, 2, P], BF16) for i in range(2)]; ks = [sb1(f"ks{i}", [P, 2, P], BF16) for i in range(2)]
    ATm = [sb1(f"ATm{i}", [P, P], BF16) for i in range(2)]; ktok = [sb1(f"ktok{i}", [P, 256], BF16) for i in range(2)]
    mixtok = [sb1(f"mixtok{i}", [P, 2048], BF16) for i in range(2)]
    gsb = sb1("gsb", [P, 16])
    nls = sb1("nls", [P, 2]); ival = sb1("ival", [P, 2])

    T.dma("sync", GN[:], gn_d.partition_broadcast(P), writes=["GN"])
    T.op("pool", lambda e: e.memset(S32[:], 0.0), writes=[("S32", i) for i in range(8)])
    T.op("pool", lambda e: e.memset(Sbf[:], 0.0), writes=[("Sbf", i) for i in range(8)])
    T.op("pool", lambda e: e.memset(n32[:], 0.0), writes=[("n32", i) for i in range(4)])
    T.op("pool", lambda e: e.memset(nbf[:], 0.0), writes=[("nbf", i) for i in range(4)])
    T.op("pool", lambda e: e.memset(pre[:], 0.0), writes=[("pre", i) for i in range(8)])
    T.op("pool", lambda e: e.memset(mixtok[0][:], 0.0), writes=["mixtok0"])
    T.dma("sync", send[0:128, :], mixtok[0][:], reads=["mixtok0"], writes=[("send", 0)])

    def load_slab(src_ap, nk, ncol):
        si = next_slab()
        dst = slabs[si][:, 0:nk * ncol].rearrange("p (k c) -> p k c", k=nk)
        T.dma("pool", dst, src_ap.rearrange("(k p) c -> p k c", p=P), writes=[("slab", si)])
        return si, dst

    def emit_xT(it, tt):
        r0 = it * 512 + tt * 128
        T.dma("sync", xb[:], x1[r0:r0 + 128, :], writes=["xb"])
        for q in range(8):
            T.op("act", lambda e, q=q: e.activation(out=junk[:, 0:512], in_=xb[:, q * 512:(q + 1) * 512], func=ACT.Square,
                                                    accum_out=sm[:, 8 + q:9 + q]),
                 reads=["xb"], writes=["junk", "sm8"])
        yield
        T.op("dve", lambda e: e.reduce_sum(out=sm[:, 0:1], in_=sm[:, 8:16], axis=mybir.AxisListType.X),
             reads=["sm8"], writes=["sm0"])
        rstd_from_ssq(sm[:, 0:1], sm[:, 0:1], D, "sm0")
        yield
        T.op("dve", lambda e: e.tensor_scalar(out=xb[:], in0=xb[:], scalar1=sm[:, 0:1], scalar2=None, op0=ALU.mult),
             reads=["xb", "sm0"], writes=["xb"])
        for q in range(8):
            yield
            bank = q % 2
            for j in range(4):
                kd = 4 * q + j
                T.op("pe", lambda e, kd=kd, j=j, bank=bank: e.transpose(
                    out=ps[bank][:, j * 128:(j + 1) * 128], in_=xb[:, kd * 128:(kd + 1) * 128], identity=ident_f[:]),
                    reads=["xb", "ident_f"], writes=[("ps", bank)])
            for j in range(4):
                kd = 4 * q + j
                if bank % 2 == 0:
                    T.op("act", lambda e, kd=kd, j=j, bank=bank, tt=tt: e.activation(
                        out=AT[:, kd, tt * 128:(tt + 1) * 128], in_=ps[bank][:, j * 128:(j + 1) * 128],
                        func=ACT.Copy, scale=g1t[:, kd:kd + 1]),
                        reads=[("ps", bank), "g1t"], writes=[("AT", tt, kd)])
                else:
                    T.op("dve", lambda e, kd=kd, j=j, bank=bank, tt=tt: e.tensor_scalar(
                        out=AT[:, kd, tt * 128:(tt + 1) * 128], in0=ps[bank][:, j * 128:(j + 1) * 128],
                        scalar1=g1t[:, kd:kd + 1], scalar2=None, op0=ALU.mult),
                        reads=[("ps", bank), "g1t"], writes=[("AT", tt, kd)])

    stopped = False
    pending = []
    ncc = [0]

    def flush_cc():
        while pending:
            p = pending.pop(0)
            ncc[0] += 1
            T.custom("pool", lambda e, p=p: e.collective_compute(
                "AllGather", ALU.bypass, replica_groups=[[0, 1], [2, 3], [4, 5], [6, 7]],
                ins=[send[p * 256:(p + 1) * 256, :]], outs=[gath[p * 512:(p + 1) * 512, :]]),
                ("cc", ncc[0]), reads=[("send", p * 256), ("send", p * 256 + 128)], writes=[("gath", p)])

    try:
      ck(1)
      for it in range(8):
          if it == 0:
              for tt in range(4):
                  for _ in emit_xT(0, tt):
                      pass

          ck(2)
          for cg in range(4):
              pb = 4 * (cg % 2)
              for sl in range(8):
                  si, slab = load_slab(wfm[sl * 512:(sl + 1) * 512, cg * 512:(cg + 1) * 512], 4, 512)
                  for k4 in range(4):
                      kd = sl * 4 + k4
                      for j in range(4):
                          T.op("pe", lambda e, slab=slab, k4=k4, j=j, kd=kd, pb=pb: e.matmul(
                              ps[pb + j][:, :], lhsT=slab[:, k4, j * 128:(j + 1) * 128], rhs=AT[:, kd, :],
                              start=(kd == 0), stop=(kd == KD - 1)),
                              reads=[("slab", si)] + [("AT", t_, kd) for t_ in range(4)], writes=[("ps", pb + j)])
              for j in range(4):
                  ct = (cg % 2) * 4 + j
                  if cg < 2:
                      T.op("act" if j % 2 else "dve",
                           (lambda e, ct=ct, j=j, pb=pb: e.activation(out=pre[:, ct, 3:515], in_=ps[pb + j][:, :], func=ACT.Copy)) if j % 2 else
                           (lambda e, ct=ct, j=j, pb=pb: e.tensor_copy(out=pre[:, ct, 3:515], in_=ps[pb + j][:, :])),
                           reads=[("ps", pb + j)], writes=[("pre", ct)])
                  else:
                      T.op("act" if j % 2 else "dve",
                           (lambda e, ct=ct, j=j, pb=pb: e.activation(out=gqk[:, ct, :], in_=ps[pb + j][:, :], func=ACT.Copy)) if j % 2 else
                           (lambda e, ct=ct, j=j, pb=pb: e.tensor_copy(out=gqk[:, ct, :], in_=ps[pb + j][:, :])),
                           reads=[("ps", pb + j)], writes=[("gqk", ct)])

          for kd in range(KD):
              T.op("pe", lambda e, kd=kd: e.matmul(ps[0][0:16, :], lhsT=wsm_sb[:, kd, 0:16], rhs=AT[:, kd, :],
                                                    start=(kd == 0), stop=(kd == KD - 1)),
                   reads=["wsm"] + [("AT", t_, kd) for t_ in range(4)], writes=[("ps", 0)])
          T.op("act", lambda e: e.activation(out=gaT[:, :], in_=ps[0][0:16, :], func=ACT.Copy),
               reads=[("ps", 0)], writes=["gaT"])
          for tt in range(4):
              for kd in range(KD):
                  T.op("pe", lambda e, kd=kd, tt=tt: e.matmul(ps[1][:, tt * 4:tt * 4 + 4], lhsT=AT[:, kd, tt * 128:(tt + 1) * 128],
                                                             rhs=wsm_sb[:, kd, 16:20], start=(kd == 0), stop=(kd == KD - 1)),
                       reads=["wsm", ("AT", tt, kd)], writes=[("ps", 1)])
          T.op("dve", lambda e: e.tensor_copy(out=gsb[:, :], in_=ps[1][:, 0:16]), reads=[("ps", 1)], writes=["gsb"])

          ck(3)
          for cg in range(8):
              pb = 4 * (cg % 2)
              for sl in range(8):
                  si, slab = load_slab(wtm[sl * 512:(sl + 1) * 512, cg * 512:(cg + 1) * 512], 4, 512)
                  for k4 in range(4):
                      kd = sl * 4 + k4
                      for tt in range(4):
                          T.op("pe", lambda e, slab=slab, k4=k4, tt=tt, kd=kd, pb=pb: e.matmul(
                              ps[pb + tt][:, :], lhsT=AT[:, kd, tt * 128:(tt + 1) * 128], rhs=slab[:, k4, :],
                              start=(kd == 0), stop=(kd == KD - 1)),
                              reads=[("slab", si), ("AT", tt, kd)], writes=[("ps", pb + tt)])
              for tt in range(4):
                  if cg < 4:
                      T.op("act" if tt % 2 else "dve",
                           (lambda e, cg=cg, tt=tt, pb=pb: e.activation(out=V[:, tt, cg * 512:(cg + 1) * 512], in_=ps[pb + tt][:, :], func=ACT.Copy)) if tt % 2 else
                           (lambda e, cg=cg, tt=tt, pb=pb: e.tensor_copy(out=V[:, tt, cg * 512:(cg + 1) * 512], in_=ps[pb + tt][:, :])),
                           reads=[("ps", pb + tt)], writes=[("V", tt)])
                  else:
                      c0 = (cg - 4) * 512
                      fn_ = ACT.Sigmoid if cg < 6 else ACT.Silu
                      tb = tmpcs[tt % 2]; tbk = ("tmpc", "tmpc2")[tt % 2]
                      T.op("act", lambda e, c0=c0, tt=tt, fn_=fn_, pb=pb, tb=tb: e.activation(out=tb[:, :], in_=ps[pb + tt][:, :], func=fn_),
                           reads=[("ps", pb + tt)], writes=[tbk])
                      T.op("dve", lambda e, c0=c0, tt=tt, tb=tb: e.tensor_tensor(out=OG[:, tt, c0:c0 + 512], in0=tb[:, :],
                                                                           in1=GN[:, c0:c0 + 512], op=ALU.mult),
                           reads=[tbk, "GN"], writes=[("OG", tt)])

          ck(4)
          for ct in range(8):
              T.op("dve", lambda e, ct=ct: e.tensor_scalar(out=tmpc[:, :], in0=pre[:, ct, 0:512], scalar1=cwb[:, ct, 0:1],
                                                           scalar2=cwb[:, ct, 4:5], op0=ALU.mult, op1=ALU.add),
                   reads=[("pre", ct), "cwb"], writes=["tmpc"])
              for j in (1, 2, 3):
                  T.op("dve", lambda e, ct=ct, j=j: e.scalar_tensor_tensor(out=tmpc[:, :], in0=pre[:, ct, j:j + 512],
                                                                           scalar=cwb[:, ct, j:j + 1], in1=tmpc[:, :],
                                                                           op0=ALU.mult, op1=ALU.add),
                       reads=[("pre", ct), "cwb", "tmpc"], writes=["tmpc"])
              T.op("act", lambda e, ct=ct: e.activation(out=qk[:, ct, :], in_=tmpc[:, :], func=ACT.Silu),
                   reads=["tmpc"], writes=[("qk", ct)])
              T.op("pool", lambda e, ct=ct: e.tensor_copy(out=pre[:, ct, 0:3], in_=pre[:, ct, 512:515]),
                   reads=[("pre", ct)], writes=[("pre", ct)])

          for hd in range(4):
              T.op("pe", lambda e, hd=hd: e.matmul(ps[4][:, :], lhsT=aw2[0:16, hd * 128:(hd + 1) * 128], rhs=gaT[0:16, :],
                                                    start=True, stop=True),
                   reads=["aw2", "gaT"], writes=[("ps", 4)])
              T.op("act", lambda e, hd=hd: e.activation(out=spt[:, :], in_=ps[4][:, :], func=ACT.Exp, scale=-1.0,
                                                         bias=nab[:, hd:hd + 1]),
                   reads=[("ps", 4), "nab"], writes=["spt"])
              T.op("act", lambda e: e.activation(out=spt[:, :], in_=spt[:, :], func=ACT.Ln, bias=1.0),
                   reads=["spt"], writes=["spt"])
              T.op("dve", lambda e: e.tensor_tensor_scan(out=csp[:, :], data0=segm[:, :], data1=spt[:, :], initial=0.0,
                                                         op0=ALU.mult, op1=ALU.add),
                   reads=["spt", "segm"], writes=["csp"])
              T.op("act", lambda e, hd=hd: e.activation(out=EQg[:, hd, :], in_=csp[:, :], func=ACT.Exp, scale=-1.0 / 16.0),
                   reads=["csp"], writes=[("EQg", hd)])
              T.op("act", lambda e, hd=hd: e.activation(out=EKg[:, hd, :], in_=csp[:, :], func=ACT.Exp, scale=1.0 / 16.0),
                   reads=["csp"], writes=[("EKg", hd)])

          ck(5)
          flush_cc()
          for tt in range(4):
              c = it * 4 + tt
              tsl = slice(tt * 128, (tt + 1) * 128)
              mt = mixtok[c % 2]
              mtk = f"mixtok{c % 2}"
              for h in range(2):
                  T.op("act", lambda e, h=h, tt=tt: e.activation(out=nls[:, h:h + 1], in_=gsb[:, tt * 4 + 2 + h:tt * 4 + 3 + h],
                                                                 func=ACT.Exp, scale=-1.0, bias=ngb[:, 2 + h:3 + h]),
                       reads=["gsb", "ngb"], writes=["nls"])
              T.op("act", lambda e: e.activation(out=nls[:, :], in_=nls[:, :], func=ACT.Ln, bias=1.0),
                   reads=["nls"], writes=["nls"])
              T.op("dve", lambda e, tt=tt: e.tensor_tensor(out=ival[:, :], in0=gsb[:, tt * 4:tt * 4 + 2], in1=gb[:, 0:2], op=ALU.add),
                   reads=["gsb", "gb"], writes=["ival"])
              for h in range(2):
                  T.op("dve", lambda e, h=h: e.tensor_scalar(out=rb[:, :], in0=tri[:, :], scalar1=nls[:, h:h + 1], scalar2=None,
                                                             op0=ALU.mult), reads=["tri", "nls"], writes=["rb"])
                  T.op("dve", lambda e, h=h: e.scalar_tensor_tensor(out=ru[:, :], in0=ident_f[:, :], scalar=ival[:, h:h + 1],
                                                                    in1=rb[:, :], op0=ALU.mult, op1=ALU.add),
                       reads=["ident_f", "ival", "rb"], writes=["ru"])
                  T.op("pe", lambda e: e.matmul(ps[4][:, 0:128], lhsT=ones_f[:, :], rhs=rb[:, :], start=True, stop=True),
                       reads=["ones_f", "rb"], writes=[("ps", 4)])
                  T.op("pe", lambda e: e.matmul(ps[4][:, 128:256], lhsT=ones_f[:, :], rhs=ru[:, :], start=True, stop=True),
                       reads=["ones_f", "ru"], writes=[("ps", 4)])
                  T.op("act", lambda e, h=h: e.activation(out=EQm[:, h, :], in_=ps[4][:, 0:128], func=ACT.Exp, scale=-1.0),
                       reads=[("ps", 4)], writes=[("EQm", h)])
                  T.op("act", lambda e, h=h: e.activation(out=EKm[:, h, :], in_=ps[4][:, 128:256], func=ACT.Exp),
                       reads=[("ps", 4)], writes=[("EKm", h)])

              def stage(hi, st, tt=tt, mt=mt, mtk=mtk, tsl=tsl):
                  ism = hi < 2
                  h = hi % 2
                  s_ = hi % 2
                  bA, bN = ((5, 6), (7, 3))[s_]
                  qsk, ksk, atk, ktk = ("qs", s_), ("ks", s_), ("ATm", s_), ("ktok", s_)
                  smk = f"smo{s_}"
                  c0 = 16 + 4 * s_
                  vsl = slice(hi * 512, (hi + 1) * 512)
                  dcol = 300 + 2 * hi
                  if st == 1:
                      for dt in range(2):
                          if ism:
                              qsrc = qk[:, 2 * h + dt, tsl]; ksrc = qk[:, 4 + 2 * h + dt, tsl]
                              eq = EQm[:, h, :]; ek = EKm[:, h, :]
                              rq = [("qk", 2 * h + dt), ("EQm", h)]; rk = [("qk", 4 + 2 * h + dt), ("EKm", h)]
                          else:
                              qsrc = gqk[:, 2 * h + dt, tsl]; ksrc = gqk[:, 4 + 2 * h + dt, tsl]
                              eq = EQg[:, 2 * h + dt, tsl]; ek = EKg[:, 2 * h + dt, tsl]
                              rq = [("gqk", 2 * h + dt), ("EQg", 2 * h + dt)]; rk = [("gqk", 4 + 2 * h + dt), ("EKg", 2 * h + dt)]
                          T.op("dve", lambda e, qsrc=qsrc, eq=eq, dt=dt: e.scalar_tensor_tensor(
                              out=qs[s_][:, dt, :], in0=qsrc, scalar=0.0625, in1=eq, op0=ALU.mult, op1=ALU.mult),
                              reads=rq, writes=[qsk])
                          T.op("pool", lambda e, ksrc=ksrc, ek=ek, dt=dt: e.tensor_tensor(
                              out=ks[s_][:, dt, :], in0=ksrc, in1=ek, op=ALU.mult), reads=rk, writes=[ksk])
                  elif st == 2:
                      for dt in range(2):
                          T.op("pe", lambda e, dt=dt: e.matmul(ps[bA][:, 0:128], lhsT=ks[s_][:, dt, :], rhs=qs[s_][:, dt, :],
                                                                start=(dt == 0), stop=(dt == 1)),
                               reads=[ksk, qsk], writes=[("ps", bA)])
                      for dt in range(2):
                          T.op("pe", lambda e, dt=dt: e.matmul(ps[bA][:, 128 + dt * 128:256 + dt * 128], lhsT=ks[s_][:, dt, :],
                                                                rhs=ident_b[:, :], start=True, stop=True),
                               reads=[ksk, "ident_b"], writes=[("ps", bA)])
                  elif st == 3:
                      T.op("dve", lambda e: e.tensor_tensor(out=ATm[s_][:, :], in0=ps[bA][:, 0:128], in1=tri[:, :], op=ALU.mult),
                           reads=[("ps", bA), "tri"], writes=[atk])
                      T.op("act", lambda e: e.activation(out=ktok[s_][:, :], in_=ps[bA][:, 128:384], func=ACT.Copy),
                           reads=[("ps", bA)], writes=[ktk])
                  elif st == 4:
                      T.op("pe", lambda e: e.matmul(ps[bN][:, :], lhsT=ATm[s_][:, :], rhs=V[:, tt, vsl], start=True, stop=False),
                           reads=[atk, ("V", tt)], writes=[("ps", bN)])
                      for dt in range(2):
                          T.op("pe", lambda e, dt=dt: e.matmul(ps[bN][:, :], lhsT=qs[s_][:, dt, :], rhs=Sbf[:, hi * 2 + dt, :],
                                                                start=False, stop=(dt == 1)),
                               reads=[qsk, ("Sbf", hi * 2 + dt)], writes=[("ps", bN)])
                      if ism:
                          T.op("pe", lambda e: e.matmul(ps[4][:, dcol:dcol + 2], lhsT=ATm[s_][:, :], rhs=ones_b[:, :], start=True, stop=False),
                               reads=[atk, "ones_b"], writes=[("ps", 4)])
                          for dt in range(2):
                              T.op("pe", lambda e, dt=dt: e.matmul(ps[4][:, dcol:dcol + 2], lhsT=qs[s_][:, dt, :], rhs=nbf[:, h * 2 + dt, :],
                                                                    start=False, stop=(dt == 1)),
                                   reads=[qsk, ("nbf", h * 2 + dt)], writes=[("ps", 4)])
                  elif st == 5:
                      T.op("act", lambda e: e.activation(out=junk[:, 0:512], in_=ps[bN][:, :], func=ACT.Square, accum_out=sm[:, c0 + 1:c0 + 2]),
                           reads=[("ps", bN)], writes=["junk", smk])
                      a1 = sm[:, c0 + 1:c0 + 2]; a2 = sm[:, c0 + 2:c0 + 3]; a3 = sm[:, c0 + 3:c0 + 4]
                      if ism:
                          T.op("dve", lambda e: e.tensor_copy(out=a3, in_=ps[4][:, dcol:dcol + 1]), reads=[("ps", 4)], writes=[smk])
                          T.op("dve", lambda e: e.tensor_tensor(out=a2, in0=a3, in1=a3, op=ALU.mult), reads=[smk], writes=[smk])
                          T.op("dve", lambda e: e.tensor_scalar(out=a2, in0=a2, scalar1=1.0, scalar2=None, op0=ALU.max), reads=[smk], writes=[smk])
                          T.op("dve", lambda e: e.reciprocal(out=a2, in_=a2), reads=[smk], writes=[smk])
                          T.op("dve", lambda e: e.tensor_tensor(out=a1, in0=a1, in1=a2, op=ALU.mult), reads=[smk], writes=[smk])
                          T.op("dve", lambda e: e.tensor_scalar(out=a1, in0=a1, scalar1=1.0 / 512, scalar2=EPS, op0=ALU.mult, op1=ALU.add),
                               reads=[smk], writes=[smk])
                          T.op("dve", lambda e: e.reciprocal(out=a1, in_=a1), reads=[smk], writes=[smk])
                          T.op("dve", lambda e: e.tensor_tensor(out=a1, in0=a1, in1=a2, op=ALU.mult), reads=[smk], writes=[smk])
                          T.op("act", lambda e: e.activation(out=a1, in_=a1, func=ACT.Sqrt), reads=[smk], writes=[smk])
                      else:
                          rstd_from_ssq(a1, a1, 512, smk)
                      T.op("dve", lambda e: e.scalar_tensor_tensor(
                          out=mt[:, vsl], in0=ps[bN][:, :], scalar=a1, in1=OG[:, tt, vsl], op0=ALU.mult, op1=ALU.mult),
                          reads=[("ps", bN), smk, ("OG", tt)], writes=[mtk])
                  elif st == 6:
                      for dt in range(2):
                          si_ = hi * 2 + dt
                          if ism:
                              eL = EQm[:, h, 127:128]; reL = [("EQm", h)]
                          else:
                              eL = EQg[:, 2 * h + dt, tt * 128 + 127:tt * 128 + 128]; reL = [("EQg", 2 * h + dt)]
                          T.op("pe", lambda e, dt=dt: e.matmul(ps[bA][:, :], lhsT=ktok[s_][:, dt * 128:(dt + 1) * 128],
                                                                rhs=V[:, tt, vsl], start=True, stop=True),
                               reads=[ktk, ("V", tt)], writes=[("ps", bA)])
                          T.op("dve", lambda e, si_=si_: e.tensor_tensor(out=S32[:, si_, :], in0=ps[bA][:, :], in1=S32[:, si_, :], op=ALU.add),
                               reads=[("ps", bA), ("S32", si_)], writes=[("S32", si_)])
                          T.op("act", lambda e, si_=si_, eL=eL: e.activation(out=S32[:, si_, :], in_=S32[:, si_, :], func=ACT.Copy, scale=eL),
                               reads=[("S32", si_)] + reL, writes=[("S32", si_)])
                          T.op("pool", lambda e, si_=si_: e.tensor_copy(out=Sbf[:, si_, :], in_=S32[:, si_, :]),
                               reads=[("S32", si_)], writes=[("Sbf", si_)])
                          if ism:
                              ni = h * 2 + dt
                              ncol = 320 + 2 * ni
                              T.op("pe", lambda e, dt=dt, ncol=ncol: e.matmul(ps[4][:, ncol:ncol + 2], lhsT=ktok[s_][:, dt * 128:(dt + 1) * 128],
                                                                               rhs=ones_b[:, :], start=True, stop=True),
                                   reads=[ktk, "ones_b"], writes=[("ps", 4)])
                              T.op("dve", lambda e, ni=ni, ncol=ncol: e.tensor_tensor(out=n32[:, ni, :], in0=ps[4][:, ncol:ncol + 2], in1=n32[:, ni, :], op=ALU.add),
                                   reads=[("ps", 4), ("n32", ni)], writes=[("n32", ni)])
                              T.op("dve", lambda e, ni=ni, eL=eL: e.tensor_scalar(out=n32[:, ni, :], in0=n32[:, ni, :], scalar1=eL, scalar2=None,
                                                                                  op0=ALU.mult),
                                   reads=[("n32", ni)] + reL, writes=[("n32", ni)])
                              T.op("dve", lambda e, ni=ni: e.tensor_copy(out=nbf[:, ni, :], in_=n32[:, ni, :]),
                                   reads=[("n32", ni)], writes=[("nbf", ni)])

              gen = emit_xT(it + 1, tt) if it + 1 < 8 else iter(())
              for pair in ((0, 1), (2, 3)):
                  for st in range(1, 7):
                      for hi in pair:
                          stage(hi, st)
                      next(gen, None)
              for _ in gen:
                  pass
              ck(6)
              if c <= 15:
                  T.dma("sync", send[128 + 128 * c:256 + 128 * c, :], mt[:], reads=[mtk], writes=[("send", 128 + 128 * c)])
              if c >= 15:
                  T.dma("sync", send[SH + 128 * (c - 15):SH + 128 * (c - 14), :], mt[:], reads=[mtk],
                        writes=[("send", SH + 128 * (c - 15))])
              if c % 2 == 0 and c <= 14:
                  pending.append(c // 2)
              elif c == 15:
                  pending.append(8)
              elif c % 2 == 1 and c >= 17:
                  pending.append((c + 1) // 2)

          ck(7)
      ck(8)
    except StopEmit:
        stopped = True
        if os.environ.get("KCC"):
            stopped = False
            pending[:] = list(range(17))
    if not stopped:
        flush_cc()
    if KDBG:
        for p_ in range(17):
            T.dma("sync", dbg_send[p_ * 256:(p_ + 1) * 256, :], send[p_ * 256:(p_ + 1) * 256, :],
                  reads=[("send", p_ * 256), ("send", p_ * 256 + 128)], writes=["dbg_send"])
    T.barrier()
    phase1_lists = T.lists
    T.lists = {k: [] for k in phase1_lists}

    def replay(e, lst):
        for waits, fn, inc in lst:
            for k, v in waits:
                e.wait_ge(sems[k], v)
            if fn is not None:
                ins = fn(e)
                ins.then_inc(sems[inc[0]], inc[1])

    def run_block(lists):
        with nc.Block() as block:
            @block.tensor
            def _(e):
                replay(e, lists["pe"])

            @block.scalar
            def _(e):
                replay(e, lists["act"])

            @block.vector
            def _(e):
                replay(e, lists["dve"])

            @block.gpsimd
            def _(e):
                replay(e, lists["pool"])

            @block.sync
            def _(e):
                replay(e, lists["sync"])

    run_block(phase1_lists)
    p1.close()

    x1buf = sb("x1buf", [P, 4, D]); G3 = sb("G3", [P, D])
    slabs.append(sb("slab3", [P, 2048], BF16))
    slabs.append(sb("slab4", [P, 2048], BF16))
    act_sb = sb("act_sb", [P, 44, 512], BF16)
    cand = [sb(f"cand{i}", [P, 2048], BF16) for i in range(2)]
    mixrow = sb("mixrow", [P, D], BF16)
    gbuf = sb("gbuf", [P, 514]); gc = sb("gc", [P, 512])
    ghalo = sb("ghalo", [P, NF, 2]); rbc = sb("rbc", [P, P]); dgm = sb("dgm", [P, P])

    T.dma("sync", G3[:], g3_d.partition_broadcast(P), writes=["G3"])
    set_i = [0]

    def next_set():
        s_ = set_i[0] % 2
        set_i[0] += 1
        return s_

    try:
        if stopped or KSTOP == 9 or os.environ.get("KCC") == "1":
            raise StopEmit()
        def emit_mix_prep(r0_, tt, bb):
            r = r0_ + tt * 128
            for rk in range(2):
                for s_ in range(2):
                    gi_ = s_ * SH + r
                    gr_ = (gi_ // 256) * 512 + rk * 256 + (gi_ % 256)
                    T.dma("sync", cand[s_][:], gath[gr_:gr_ + 128, :],
                          reads=[("gath", gi_ // 256)], writes=[f"cand{s_}"])
                T.op("dve", lambda e, rk=rk: e.tensor_scalar(out=mixrow[:, rk * 2048:(rk + 1) * 2048], in0=cand[0][:], scalar1=sel[:, 0:1],
                                                             scalar2=None, op0=ALU.mult),
                     reads=["cand0", "sel"], writes=["mixrow"])
                T.op("dve", lambda e, rk=rk: e.scalar_tensor_tensor(out=mixrow[:, rk * 2048:(rk + 1) * 2048], in0=cand[1][:], scalar=sel[:, 1:2],
                                                                    in1=mixrow[:, rk * 2048:(rk + 1) * 2048], op0=ALU.mult, op1=ALU.add),
                     reads=["cand1", "sel", "mixrow"], writes=["mixrow"])
            for q in range(8):
                bank = bb + (q % 2)
                for j in range(4):
                    kc = 4 * q + j
                    T.op("pe", lambda e, kc=kc, j=j, bank=bank: e.matmul(ps[bank][:, j * 128:(j + 1) * 128],
                                                                          lhsT=mixrow[:, kc * 128:(kc + 1) * 128], rhs=ident_b[:, :],
                                                                          start=True, stop=True),
                         reads=["mixrow", "ident_b"], writes=[("ps", bank)])
                for j in range(4):
                    kc = 4 * q + j
                    T.op("act" if bank % 2 else "dve",
                         (lambda e, kc=kc, j=j, bank=bank, tt=tt: e.activation(out=AT[:, kc, tt * 128:(tt + 1) * 128],
                                                                                 in_=ps[bank][:, j * 128:(j + 1) * 128], func=ACT.Copy)) if bank % 2 else
                         (lambda e, kc=kc, j=j, bank=bank, tt=tt: e.tensor_copy(out=AT[:, kc, tt * 128:(tt + 1) * 128],
                                                                                  in_=ps[bank][:, j * 128:(j + 1) * 128])),
                         reads=[("ps", bank)], writes=[("AT", tt)])

        halves = [(0, 44), (44, 42)]
        for tile in range(-1, 4):
            ntt = 1 if tile < 0 else 4
            Tn = ntt * 128
            r0 = 0 if tile < 0 else 128 + 512 * tile
            for tt in range(ntt):
                r = r0 + tt * 128
                T.dma("sync", x1buf[:, tt, :], x2[r:r + 128, :], writes=[("x1buf", tt)])
                if tile <= 0:
                    emit_mix_prep(r0, tt, 0)
            ATt = [("AT", t) for t in range(ntt)]
            for dg in range(8):
                s_ = next_set()
                for sl in range(8):
                    si, slab = load_slab(wo[sl * 512:(sl + 1) * 512, dg * 512:(dg + 1) * 512], 4, 512)
                    for k4 in range(4):
                        kc = sl * 4 + k4
                        for tt in range(ntt):
                            T.op("pe", lambda e, slab=slab, k4=k4, tt=tt, kc=kc, s_=s_: e.matmul(
                                ps[s_ * 4 + tt][:, :], lhsT=AT[:, kc, tt * 128:(tt + 1) * 128], rhs=slab[:, k4, :],
                                start=(kc == 0), stop=(kc == KD - 1)),
                                reads=[("slab", si), ("AT", tt)], writes=[("ps", s_ * 4 + tt)])
                for tt in range(ntt):
                    T.op("dve", lambda e, tt=tt, dg=dg, s_=s_: e.tensor_tensor(
                        out=x1buf[:, tt, dg * 512:(dg + 1) * 512], in0=ps[s_ * 4 + tt][:, :], in1=x1buf[:, tt, dg * 512:(dg + 1) * 512], op=ALU.add),
                        reads=[("ps", s_ * 4 + tt), ("x1buf", tt)], writes=[("x1buf", tt)])
            if KDBG and tile >= 0:
                for tt in range(4):
                    T.dma("sync", dbg_x1[tile * 512 + tt * 128:tile * 512 + tt * 128 + 128, :], x1buf[:, tt, :],
                          reads=[("x1buf", tt)], writes=["dbg_x1"])
            for tt in range(ntt):
                T.op("act", lambda e, tt=tt: e.activation(out=mixrow[:, :], in_=x1buf[:, tt, :], func=ACT.Square, accum_out=sm[:, 4:5]),
                     reads=[("x1buf", tt)], writes=["mixrow", "sm4"])
                rstd_from_ssq(sm[:, 4:5], sm[:, 4:5], D, "sm4")
                T.op("dve", lambda e: e.tensor_scalar(out=dgm[:, :], in0=ident_f[:, :], scalar1=sm[:, 4:5], scalar2=None, op0=ALU.mult),
                     reads=["ident_f", "sm4"], writes=["dgm"])
                T.op("pe", lambda e: e.matmul(ps[7][:, 0:128], lhsT=ones_f[:, :], rhs=dgm[:, :], start=True, stop=True),
                     reads=["ones_f", "dgm"], writes=[("ps", 7)])
                T.op("act", lambda e: e.activation(out=rbc[:, :], in_=ps[7][:, 0:128], func=ACT.Copy), reads=[("ps", 7)], writes=["rbc"])
                for q in range(8):
                    bank = 4 + (q % 2)
                    for j in range(4):
                        kd = 4 * q + j
                        T.op("pe", lambda e, kd=kd, j=j, bank=bank, tt=tt: e.transpose(
                            out=ps[bank][:, j * 128:(j + 1) * 128], in_=x1buf[:, tt, kd * 128:(kd + 1) * 128], identity=ident_f[:]),
                            reads=[("x1buf", tt), "ident_f"], writes=[("ps", bank)])
                    for j in range(4):
                        kd = 4 * q + j
                        T.op("dve", lambda e, kd=kd, j=j, bank=bank, tt=tt: e.scalar_tensor_tensor(
                            out=AT[:, kd, tt * 128:(tt + 1) * 128], in0=ps[bank][:, j * 128:(j + 1) * 128], scalar=g2t[:, kd:kd + 1],
                            in1=rbc[:, :], op0=ALU.mult, op1=ALU.mult),
                            reads=[("ps", bank), "g2t", "rbc"], writes=[("AT", tt)])
            if tile < 0:
                for fa in range(0, NF, 4):
                    nfg = min(4, NF - fa)
                    s_ = next_set()
                    for sl in range(8):
                        si, slab = load_slab(wg[sl * 512:(sl + 1) * 512, fa * 128:(fa + nfg) * 128], 4, nfg * 128)
                        for k4 in range(4):
                            kd = sl * 4 + k4
                            for u in range(nfg):
                                bk = s_ * 4 + u
                                T.op("pe", lambda e, slab=slab, k4=k4, u=u, kd=kd, bk=bk: e.matmul(
                                    ps[bk][:, 0:128], lhsT=slab[:, k4, u * 128:(u + 1) * 128], rhs=AT[:, kd, 0:128],
                                    start=(kd == 0), stop=(kd == KD - 1)),
                                    reads=[("slab", si)] + ATt, writes=[("ps", bk)])
                    for u in range(nfg):
                        bk = s_ * 4 + u
                        T.op("dve" if u % 2 else "act",
                             (lambda e, f=fa + u, bk=bk: e.tensor_copy(out=ghalo[:, f, :], in_=ps[bk][:, 126:128])) if u % 2 else
                             (lambda e, f=fa + u, bk=bk: e.activation(out=ghalo[:, f, :], in_=ps[bk][:, 126:128], func=ACT.Copy)),
                             reads=[("ps", bk)], writes=[("ghalo", fa + u)])
                ck(10)
                continue
            for (f0, nfh) in halves:
                for fa in range(f0, f0 + nfh, 2):
                    s_ = next_set()
                    for wi, W in enumerate((wg, wu)):
                        if tile < 0 and wi == 1:
                            continue
                        for sl in range(4):
                            si, slab = load_slab(W[sl * 1024:(sl + 1) * 1024, fa * 128:(fa + 2) * 128], 8, 256)
                            for k8 in range(8):
                                kd = sl * 8 + k8
                                for u in range(2):
                                    bk = s_ * 4 + wi * 2 + u
                                    T.op("pe", lambda e, slab=slab, k8=k8, u=u, kd=kd, bk=bk, Tn=Tn: e.matmul(
                                        ps[bk][:, 0:Tn], lhsT=slab[:, k8, u * 128:(u + 1) * 128], rhs=AT[:, kd, 0:Tn],
                                        start=(kd == 0), stop=(kd == KD - 1)),
                                        reads=[("slab", si)] + ATt, writes=[("ps", bk)])
                    for u in range(2):
                        f = fa + u
                        bg = s_ * 4 + u
                        bu = s_ * 4 + 2 + u
                        if tile < 0:
                            T.op("dve", lambda e, f=f, bg=bg, Tn=Tn: e.tensor_copy(out=ghalo[:, f, :], in_=ps[bg][:, Tn - 2:Tn]),
                                 reads=[("ps", bg)], writes=["ghalo"])
                            continue
                        T.op("act", lambda e, bg=bg: e.activation(out=gbuf[:, 2:514], in_=ps[bg][:, :], func=ACT.Copy),
                             reads=[("ps", bg)], writes=["gbuf"])
                        T.op("pool", lambda e, f=f: e.tensor_copy(out=gbuf[:, 0:2], in_=ghalo[:, f, :]), reads=[("ghalo", f)], writes=["gbuf"])
                        T.op("dve", lambda e, f=f: e.tensor_scalar(out=gc[:, :], in0=gbuf[:, 0:512], scalar1=fcw[:, f, 0:1], scalar2=fcw[:, f, 3:4],
                                                                   op0=ALU.mult, op1=ALU.add), reads=["gbuf", "fcw"], writes=["gc"])
                        for j in (1, 2):
                            T.op("dve", lambda e, f=f, j=j: e.scalar_tensor_tensor(out=gc[:, :], in0=gbuf[:, j:j + 512], scalar=fcw[:, f, j:j + 1],
                                                                                   in1=gc[:, :], op0=ALU.mult, op1=ALU.add),
                                 reads=["gbuf", "fcw", "gc"], writes=["gc"])
                        T.op("pool", lambda e, f=f: e.tensor_copy(out=ghalo[:, f, :], in_=gbuf[:, 512:514]), reads=["gbuf"], writes=[("ghalo", f)])
                        T.op("act", lambda e: e.activation(out=gc[:, :], in_=gc[:, :], func=ACT.Silu), reads=["gc"], writes=["gc"])
                        T.op("dve", lambda e, f=f, f0=f0, bu=bu: e.tensor_tensor(out=act_sb[:, f - f0, :], in0=gc[:, :], in1=ps[bu][:, :], op=ALU.mult),
                             reads=["gc", ("ps", bu)], writes=[("act", f - f0)])
                if tile < 0:
                    continue
                for dg in range(8):
                    s_ = next_set()
                    for fl0 in range(0, nfh, 4):
                        nk = min(4, nfh - fl0)
                        si, slab = load_slab(wd[(f0 + fl0) * 128:(f0 + fl0 + nk) * 128, dg * 512:(dg + 1) * 512], nk, 512)
                        for k4 in range(nk):
                            fl = fl0 + k4
                            for tt in range(4):
                                T.op("pe", lambda e, slab=slab, k4=k4, tt=tt, fl=fl, s_=s_, nfh=nfh: e.matmul(
                                    ps[s_ * 4 + tt][:, :], lhsT=act_sb[:, fl, tt * 128:(tt + 1) * 128], rhs=slab[:, k4, :],
                                    start=(fl == 0), stop=(fl == nfh - 1)),
                                    reads=[("slab", si), ("act", fl)], writes=[("ps", s_ * 4 + tt)])
                    if f0 > 0 and tile < 3 and dg % 2 == 1:
                        emit_mix_prep(128 + 512 * (tile + 1), (dg - 1) // 2, 4 * (1 - s_))
                    for tt in range(4):
                        T.op("dve", lambda e, tt=tt, dg=dg, s_=s_: e.tensor_tensor(
                            out=x1buf[:, tt, dg * 512:(dg + 1) * 512], in0=ps[s_ * 4 + tt][:, :], in1=x1buf[:, tt, dg * 512:(dg + 1) * 512], op=ALU.add),
                            reads=[("ps", s_ * 4 + tt), ("x1buf", tt)], writes=[("x1buf", tt)])
            if tile < 0:
                ck(10)
                continue
            for tt in range(4):
                T.op("act", lambda e, tt=tt: e.activation(out=mixrow[:, :], in_=x1buf[:, tt, :], func=ACT.Square, accum_out=sm[:, 5:6]),
                     reads=[("x1buf", tt)], writes=["mixrow", "sm5"])
                rstd_from_ssq(sm[:, 5:6], sm[:, 5:6], D, "sm5")
                T.op("dve", lambda e, tt=tt: e.scalar_tensor_tensor(out=x1buf[:, tt, :], in0=x1buf[:, tt, :], scalar=sm[:, 5:6], in1=G3[:, :],
                                                                    op0=ALU.mult, op1=ALU.mult),
                     reads=[("x1buf", tt), "sm5", "G3"], writes=[("x1buf", tt)])
                ro = tile * 512 + tt * 128
                T.dma("sync", out[ro:ro + 128, :], x1buf[:, tt, :], reads=[("x1buf", tt)], writes=["out"])

    except StopEmit:
        pass
    T.barrier()
    run_block(T.lists)
    es.close()
    return nc


_NC = None


def _prep_inputs(inp):
    f32 = np.float32
    g = lambda k: np.asarray(inp[k], dtype=f32)
    x = g("x"); w_in = g("w_in"); w_out = g("w_out")
    offs = np.cumsum([0, 1024, 1024, 2048, 2048, 4, 4, 1024, 1024, 2048, 2048, 16])
    o_mq, o_mk, o_mv, o_mo, o_mi, o_mf, o_gq, o_gk, o_gv, o_gg, o_ga = offs[:11]
    conv_w = g("mlstm_conv_w"); conv_b = g("mlstm_conv_b")
    mnorm = g("mlstm_norm_g"); gnorm = g("gla_norm_g")
    aw2 = g("gla_a_w2"); a_b = g("gla_a_b")
    i_b = g("mlstm_i_b"); f_b = g("mlstm_f_b")
    ident = np.eye(P, dtype=f32)
    tri = np.triu(np.ones((P, P), f32))
    segm = np.ones((P, 512), f32); segm[:, ::128] = 0.0
    wg = np.ascontiguousarray(g("w_ffn_gate")); wu = np.ascontiguousarray(g("w_ffn_up")); wd = np.ascontiguousarray(g("w_ffn_down"))
    fc = np.concatenate([g("ffn_conv_w"), g("ffn_conv_b")[None, :]], 0)
    fcw = np.ascontiguousarray(fc.T.reshape(NF, P, 4).transpose(1, 0, 2))
    tk = lambda v: np.ascontiguousarray(v.reshape(KD, P).T)
    g1t = tk(g("ln1_g")); g2t = tk(g("ln2_g")); g3 = g("lnf_g")[None, :].copy()
    rows = []
    for r in range(2):
        for h in (2 * r, 2 * r + 1):
            rows.append(np.arange(h * 512, (h + 1) * 512))
        for h in (2 * r, 2 * r + 1):
            rows.append(2048 + np.arange(h * 512, (h + 1) * 512))
    wo = np.ascontiguousarray(w_out[np.concatenate(rows)])
    maps = []
    for core in range(8):
        b, gi = core // 2, core % 2
        hs = (2 * gi, 2 * gi + 1)
        c256 = lambda base: np.concatenate([np.arange(base + h * 256, base + (h + 1) * 256) for h in hs])
        c512 = lambda base: np.concatenate([np.arange(base + h * 512, base + (h + 1) * 512) for h in hs])
        wfm = np.ascontiguousarray(w_in[:, np.concatenate([c256(o_mq), c256(o_mk), c256(o_gq), c256(o_gk)])])
        wtm = np.ascontiguousarray(w_in[:, np.concatenate([c512(o_mv), c512(o_gv), c512(o_mo), c512(o_gg)])])
        wsm = np.ascontiguousarray(w_in[:, np.concatenate([np.arange(o_ga, o_ga + 16), o_mi + np.array(hs), o_mf + np.array(hs)])])
        x2 = np.zeros((SH, D), f32)
        lo = 2048 * gi - 128
        if lo < 0:
            x2[128:] = x[b, 0:2048]
        else:
            x2[:] = x[b, lo:lo + SH]
        ccols = np.concatenate([c256(0), c256(1024)])
        cw = np.concatenate([conv_w[:, ccols], conv_b[None, ccols]], 0)
        cwb = np.ascontiguousarray(cw.T.reshape(8, P, 5).transpose(1, 0, 2))
        gbv = np.array([i_b[hs[0]], i_b[hs[1]], f_b[hs[0]], f_b[hs[1]]], f32)
        gb = np.ascontiguousarray(np.tile(gbv[None, :], (P, 1)))
        acols = c256(0)
        ab = np.ascontiguousarray(a_b[acols].reshape(4, P).T)
        gn = np.concatenate([mnorm[c512(0)], gnorm[c512(0)]])[None, :].copy()
        selv = np.zeros((P, 2), f32); selv[:, gi] = 1.0
        maps.append(dict(x1=np.ascontiguousarray(x[b]), x2=x2, wfm=wfm, wtm=wtm, wsm=wsm, wo=wo, wg=wg, wu=wu, wd=wd,
                         g1t=g1t, g2t=g2t, g3=g3, gn=gn, cwb=cwb, gb=gb, aw2=np.ascontiguousarray(aw2[:, acols]), ab=ab,
                         fcw=fcw, sel=selv, ident=ident, tri=tri, segm=segm))
    return maps


def kernel(**inputs):
    global _NC
    if _NC is None:
        _NC = build_nc()
    maps = _prep_inputs(inputs)
    res = run_bass_kernel_spmd(_NC, maps, core_ids=list(range(8)))
    outs = [np.asarray(res.results[c]["out"], dtype=np.float32) for c in range(8)]
    full = np.stack([np.concatenate([outs[2 * b], outs[2 * b + 1]], 0) for b in range(4)], 0)
    return full
```

```python
import contextlib
import numpy as np
import concourse.bass as bass
import concourse.mybir as mybir
from concourse.bass_utils import run_bass_kernel_spmd

F32 = mybir.dt.float32
BF16 = mybir.dt.bfloat16
ACT = mybir.ActivationFunctionType
ALU = mybir.AluOpType

P = 128
D = 4096
KD = 32
S = 4096
DFF = 11008
NF = 86
EPS = 1e-6
SH = 2176
RING = 8
import os
KSTOP = int(os.environ.get("KSTOP", "0"))
KDBG = int(os.environ.get("KDBG", "0"))


class StopEmit(Exception):
    pass


def ck(n):
    if KSTOP == n:
        raise StopEmit()


class Tracker:
    def __init__(self):
        self.lists = {k: [] for k in ("pe", "act", "dve", "pool", "sync")}
        self.cnt = {k: 0 for k in ("pe", "act", "dve", "pool")}
        self.lastw = {}
        self.readers = {}
        self.waited = {}
        self.ring_idx = {"sync": 0, "pool": 0}
        self.final = {}

    def _deps(self, reads, writes):
        d = {}
        for r in reads:
            t = self.lastw.get(r)
            if t:
                d[t[0]] = max(d.get(t[0], 0), t[1])
        for w in writes:
            t = self.lastw.get(w)
            if t:
                d[t[0]] = max(d.get(t[0], 0), t[1])
            for k, v in self.readers.get(w, {}).items():
                d[k] = max(d.get(k, 0), v)
        return d

    def _record(self, tok, reads, writes):
        for w in writes:
            self.lastw[w] = tok
            self.readers[w] = {}
        for r in reads:
            rd = self.readers.setdefault(r, {})
            rd[tok[0]] = max(rd.get(tok[0], 0), tok[1])
        self.final[tok[0]] = max(self.final.get(tok[0], 0), tok[1])

    def _emit(self, lst, deps, fn, inc):
        waits = []
        for k, v in deps.items():
            if self.waited.get((lst, k), 0) >= v:
                continue
            self.waited[(lst, k)] = v
            waits.append((k, v))
        self.lists[lst].append((waits, fn, inc))

    def op(self, eng, fn, reads=(), writes=()):
        writes = list(writes) + [r for r in reads if isinstance(r, tuple) and r[0] == "ps"]
        deps = self._deps(reads, writes)
        if eng == "pe":
            deps.pop("pe", None)
        self.cnt[eng] += 1
        self._emit(eng, deps, fn, (eng, 1))
        self._record((eng, self.cnt[eng]), reads, writes)

    def dma(self, q, out, in_, reads=(), writes=()):
        deps = self._deps(reads, writes)
        j = self.ring_idx[q]
        self.ring_idx[q] += 1
        key = (q + "ring", j % RING)
        if j >= RING:
            deps[key] = max(deps.get(key, 0), 16 * (j // RING))
        self._emit(q, deps, lambda e: e.dma_start(out=out, in_=in_), (key, 16))
        self._record((key, 16 * (j // RING + 1)), reads, writes)

    def custom(self, lst, fn, tok, reads=(), writes=()):
        deps = self._deps(reads, writes)
        self._emit(lst, deps, fn, (tok[0], 1))
        self._record(tok, reads, writes)

    def barrier(self):
        for lst in self.lists:
            self._emit(lst, dict(self.final), None, None)


def build_nc():
    nc = bass.Bass("TRN2", target_bir_lowering=False)
    T = Tracker()

    def din(name, shape, dt=F32):
        if os.environ.get("KSMALL") and name in ("x1", "x2", "wfm", "wtm", "wo", "wg", "wu", "wd"):
            shape = [128, 128]
        return nc.dram_tensor(name, list(shape), dt, kind="ExternalInput").ap()

    x1 = din("x1", [S, D]); x2 = din("x2", [SH, D])
    wfm = din("wfm", [D, 2048]); wtm = din("wtm", [D, 4096]); wsm = din("wsm", [D, 20])
    wo = din("wo", [D, D]); wg = din("wg", [D, DFF]); wu = din("wu", [D, DFF]); wd = din("wd", [DFF, D])
    g1t_d = din("g1t", [P, KD]); g2t_d = din("g2t", [P, KD]); g3_d = din("g3", [1, D]); gn_d = din("gn", [1, 2048])
    cwb_d = din("cwb", [P, 8, 5]); gb_d = din("gb", [P, 4]); aw2_d = din("aw2", [16, 512]); ab_d = din("ab", [P, 4])
    fcw_d = din("fcw", [P, NF, 4]); sel_d = din("sel", [P, 2])
    ident_d = din("ident", [P, P]); tri_d = din("tri", [P, P]); segm_d = din("segm", [P, 512])
    out = nc.dram_tensor("out", [2048, D], F32, kind="ExternalOutput").ap()
    send = nc.dram_tensor("send", [2 * SH, 2048], BF16).ap()
    gath = nc.dram_tensor("gath", [17 * 512, 2048], BF16).ap()
    if KDBG:
        dbg_send = nc.dram_tensor("dbg_send", [2 * SH, 2048], BF16, kind="ExternalOutput").ap()
        dbg_x1 = nc.dram_tensor("dbg_x1", [2048, D], F32, kind="ExternalOutput").ap()

    es = contextlib.ExitStack()

    def sb(name, shape, dt=F32):
        return es.enter_context(nc.sbuf_tensor("sb_" + name, list(shape), dt))

    sems = {}
    for k in ("pe", "act", "dve", "pool", "cc"):
        sems[k] = es.enter_context(nc.semaphore("s_" + k))
    for q in ("sync", "pool"):
        for r in range(RING):
            sems[(q + "ring", r)] = es.enter_context(nc.semaphore(f"r_{q}{r}"))
    ps = [es.enter_context(nc.psum_tensor(f"ps{b}", [P, 512], F32)) for b in range(8)]

    ident_f = sb("ident_f", [P, P]); ident_b = sb("ident_b", [P, P], BF16)
    tri = sb("tri", [P, P]); segm = sb("segm", [P, 512])
    ones_f = sb("ones_f", [P, P]); ones_b = sb("ones_b", [P, 2], BF16)
    g1t = sb("g1t", [P, KD]); g2t = sb("g2t", [P, KD])
    cwb = sb("cwb", [P, 8, 5]); gb = sb("gb", [P, 4]); ngb = sb("ngb", [P, 4])
    ab = sb("ab", [P, 4]); nab = sb("nab", [P, 4])
    fcw = sb("fcw", [P, NF, 4]); sel = sb("sel", [P, 2])
    aw2 = sb("aw2", [16, 512], BF16)
    wsm_sb = sb("wsm_sb", [P, KD, 20], BF16)
    slabs = [sb(f"slab{i}", [P, 2048], BF16) for i in range(3)]
    sm = sb("sm", [P, 64])
    slab_i = [0]

    def next_slab():
        i = slab_i[0] % len(slabs)
        slab_i[0] += 1
        return i

    for dst, src, nm in ((ident_f, ident_d, "ident_f"), (tri, tri_d, "tri"), (segm, segm_d, "segm"),
                         (g1t, g1t_d, "g1t"), (g2t, g2t_d, "g2t"), (gb, gb_d, "gb"), (ab, ab_d, "ab"),
                         (sel, sel_d, "sel")):
        T.dma("sync", dst[:], src, writes=[nm])
    T.dma("sync", cwb[:], cwb_d, writes=["cwb"])
    T.dma("sync", fcw[:], fcw_d, writes=["fcw"])
    T.dma("pool", aw2[:], aw2_d, writes=["aw2"])
    T.dma("pool", wsm_sb[:], wsm.rearrange("(k p) c -> p k c", p=P), writes=["wsm"])
    T.op("pool", lambda e: e.memset(ones_f[:], 1.0), writes=["ones_f"])
    T.op("pool", lambda e: e.memset(ones_b[:], 1.0), writes=["ones_b"])
    T.op("dve", lambda e: e.tensor_copy(out=ident_b[:], in_=ident_f[:]), reads=["ident_f"], writes=["ident_b"])
    T.op("dve", lambda e: e.tensor_scalar(out=ngb[:], in0=gb[:], scalar1=-1.0, scalar2=None, op0=ALU.mult),
         reads=["gb"], writes=["ngb"])
    T.op("dve", lambda e: e.tensor_scalar(out=nab[:], in0=ab[:], scalar1=-1.0, scalar2=None, op0=ALU.mult),
         reads=["ab"], writes=["nab"])

    AT = sb("AT", [P, KD, 512], BF16)
    ATr = [("AT", t) for t in range(4)]

    def rstd_from_ssq(ssq_ap, dst_ap, n, key):
        T.op("dve", lambda e: e.tensor_scalar(out=dst_ap, in0=ssq_ap, scalar1=1.0 / n, scalar2=EPS,
                                              op0=ALU.mult, op1=ALU.add), reads=[key], writes=[key])
        T.op("act", lambda e: e.activation(out=dst_ap, in_=dst_ap, func=ACT.Sqrt), reads=[key], writes=[key])
        T.op("dve", lambda e: e.reciprocal(out=dst_ap, in_=dst_ap), reads=[key], writes=[key])

    p1 = contextlib.ExitStack()

    def sb1(name, shape, dt=F32):
        return p1.enter_context(nc.sbuf_tensor("sb_" + name, list(shape), dt))

    xb = sb1("xb", [P, D]); junk = sb1("junk", [P, 512], BF16)
    pre = sb1("pre", [P, 8, 515]); qk = sb1("qk", [P, 8, 512], BF16); gqk = sb1("gqk", [P, 8, 512], BF16)
    tmpc = sb1("tmpc", [P, 512]); tmpc2 = sb1("tmpc2", [P, 512]); tmpcs = [tmpc, tmpc2]
    V = sb1("V", [P, 4, 2048], BF16); OG = sb1("OG", [P, 4, 2048], BF16); GN = sb1("GN", [P, 2048])
    gaT = sb1("gaT", [16, 512], BF16)
    EQg = sb1("EQg", [P, 4, 512]); EKg = sb1("EKg", [P, 4, 512]); spt = sb1("spt", [P, 512]); csp = sb1("csp", [P, 512])
    EQm = sb1("EQm", [P, 2, P]); EKm = sb1("EKm", [P, 2, P]); rb = sb1("rb", [P, P]); ru = sb1("ru", [P, P])
    S32 = sb1("S32", [P, 8, 512]); Sbf = sb1("Sbf", [P, 8, 512], BF16)
    n32 = sb1("n32", [P, 4, 2]); nbf = sb1("nbf", [P, 4, 2], BF16)
    qs = [sb1(f"qs{i}", [P, 2, P], BF16) for i in range(2)]; ks = [sb1(f"ks{i}", [P, 2, P], BF16) for i in range(2)]
    ATm = [sb1(f"ATm{i}", [P, P], BF16) for i in range(2)]; ktok = [sb1(f"ktok{i}", [P, 256], BF16) for i in range(2)]
    mixtok = [sb1(f"mixtok{i}", [P, 2048], BF16) for i in range(2)]
    gsb = sb1("gsb", [P, 16])
    nls = sb1("nls", [P, 2]); ival = sb1("ival", [P, 2])

    T.dma("sync", GN[:], gn_d.partition_broadcast(P), writes=["GN"])
    T.op("pool", lambda e: e.memset(S32[:], 0.0), writes=[("S32", i) for i in range(8)])
    T.op("pool", lambda e: e.memset(Sbf[:], 0.0), writes=[("Sbf", i) for i in range(8)])
    T.op("pool", lambda e: e.memset(n32[:], 0.0), writes=[("n32", i) for i in range(4)])
    T.op("pool", lambda e: e.memset(nbf[:], 0.0), writes=[("nbf", i) for i in range(4)])
    T.op("pool", lambda e: e.memset(pre[:], 0.0), writes=[("pre", i) for i in range(8)])
    T.op("pool", lambda e: e.memset(mixtok[0][:], 0.0), writes=["mixtok0"])
    T.dma("sync", send[0:128, :], mixtok[0][:], reads=["mixtok0"], writes=[("send", 0)])

    def load_slab(src_ap, nk, ncol):
        si = next_slab()
        dst = slabs[si][:, 0:nk * ncol].rearrange("p (k c) -> p k c", k=nk)
        T.dma("pool", dst, src_ap.rearrange("(k p) c -> p k c", p=P), writes=[("slab", si)])
        return si, dst

    def emit_xT(it, tt):
        r0 = it * 512 + tt * 128
        T.dma("sync", xb[:], x1[r0:r0 + 128, :], writes=["xb"])
        for q in range(8):
            T.op("act", lambda e, q=q: e.activation(out=junk[:, 0:512], in_=xb[:, q * 512:(q + 1) * 512], func=ACT.Square,
                                                    accum_out=sm[:, 8 + q:9 + q]),
                 reads=["xb"], writes=["junk", "sm8"])
        yield
        T.op("dve", lambda e: e.reduce_sum(out=sm[:, 0:1], in_=sm[:, 8:16], axis=mybir.AxisListType.X),
             reads=["sm8"], writes=["sm0"])
        rstd_from_ssq(sm[:, 0:1], sm[:, 0:1], D, "sm0")
        yield
        T.op("dve", lambda e: e.tensor_scalar(out=xb[:], in0=xb[:], scalar1=sm[:, 0:1], scalar2=None, op0=ALU.mult),
             reads=["xb", "sm0"], writes=["xb"])
        for q in range(8):
            yield
            bank = q % 2
            for j in range(4):
                kd = 4 * q + j
                T.op("pe", lambda e, kd=kd, j=j, bank=bank: e.transpose(
                    out=ps[bank][:, j * 128:(j + 1) * 128], in_=xb[:, kd * 128:(kd + 1) * 128], identity=ident_f[:]),
                    reads=["xb", "ident_f"], writes=[("ps", bank)])
            for j in range(4):
                kd = 4 * q + j
                if bank % 2 == 0:
                    T.op("act", lambda e, kd=kd, j=j, bank=bank, tt=tt: e.activation(
                        out=AT[:, kd, tt * 128:(tt + 1) * 128], in_=ps[bank][:, j * 128:(j + 1) * 128],
                        func=ACT.Copy, scale=g1t[:, kd:kd + 1]),
                        reads=[("ps", bank), "g1t"], writes=[("AT", tt, kd)])
                else:
                    T.op("dve", lambda e, kd=kd, j=j, bank=bank, tt=tt: e.tensor_scalar(
                        out=AT[:, kd, tt * 128:(tt + 1) * 128], in0=ps[bank][:, j * 128:(j + 1) * 128],
                        scalar1=g1t[:, kd:kd + 1], scalar2=None, op0=ALU.mult),
                        reads=[("ps", bank), "g1t"], writes=[("AT", tt, kd)])

    stopped = False
    pending = []
    ncc = [0]

    def flush_cc():
        while pending:
            p = pending.pop(0)
            ncc[0] += 1
            T.custom("pool", lambda e, p=p: e.collective_compute(
                "AllGather", ALU.bypass, replica_groups=[[0, 1], [2, 3], [4, 5], [6, 7]],
                ins=[send[p * 256:(p + 1) * 256, :]], outs=[gath[p * 512:(p + 1) * 512, :]]),
                ("cc", ncc[0]), reads=[("send", p * 256), ("send", p * 256 + 128)], writes=[("gath", p)])

    try:
      ck(1)
      for it in range(8):
          if it == 0:
              for tt in range(4):
                  for _ in emit_xT(0, tt):
                      pass

          ck(2)
          for cg in range(4):
              pb = 4 * (cg % 2)
              for sl in range(8):
                  si, slab = load_slab(wfm[sl * 512:(sl + 1) * 512, cg * 512:(cg + 1) * 512], 4, 512)
                  for k4 in range(4):
                      kd = sl * 4 + k4
                      for j in range(4):
                          T.op("pe", lambda e, slab=slab, k4=k4, j=j, kd=kd, pb=pb: e.matmul(
                              ps[pb + j][:, :], lhsT=slab[:, k4, j * 128:(j + 1) * 128], rhs=AT[:, kd, :],
                              start=(kd == 0), stop=(kd == KD - 1)),
                              reads=[("slab", si)] + [("AT", t_, kd) for t_ in range(4)], writes=[("ps", pb + j)])
              for j in range(4):
                  ct = (cg % 2) * 4 + j
                  if cg < 2:
                      T.op("act" if j % 2 else "dve",
                           (lambda e, ct=ct, j=j, pb=pb: e.activation(out=pre[:, ct, 3:515], in_=ps[pb + j][:, :], func=ACT.Copy)) if j % 2 else
                           (lambda e, ct=ct, j=j, pb=pb: e.tensor_copy(out=pre[:, ct, 3:515], in_=ps[pb + j][:, :])),
                           reads=[("ps", pb + j)], writes=[("pre", ct)])
                  else:
                      T.op("act" if j % 2 else "dve",
                           (lambda e, ct=ct, j=j, pb=pb: e.activation(out=gqk[:, ct, :], in_=ps[pb + j][:, :], func=ACT.Copy)) if j % 2 else
                           (lambda e, ct=ct, j=j, pb=pb: e.tensor_copy(out=gqk[:, ct, :], in_=ps[pb + j][:, :])),
                           reads=[("ps", pb + j)], writes=[("gqk", ct)])

          for kd in range(KD):
              T.op("pe", lambda e, kd=kd: e.matmul(ps[0][0:16, :], lhsT=wsm_sb[:, kd, 0:16], rhs=AT[:, kd, :],
                                                    start=(kd == 0), stop=(kd == KD - 1)),
                   reads=["wsm"] + [("AT", t_, kd) for t_ in range(4)], writes=[("ps", 0)])
          T.op("act", lambda e: e.activation(out=gaT[:, :], in_=ps[0][0:16, :], func=ACT.Copy),
               reads=[("ps", 0)], writes=["gaT"])
          for tt in range(4):
              for kd in range(KD):
                  T.op("pe", lambda e, kd=kd, tt=tt: e.matmul(ps[1][:, tt * 4:tt * 4 + 4], lhsT=AT[:, kd, tt * 128:(tt + 1) * 128],
                                                             rhs=wsm_sb[:, kd, 16:20], start=(kd == 0), stop=(kd == KD - 1)),
                       reads=["wsm", ("AT", tt, kd)], writes=[("ps", 1)])
          T.op("dve", lambda e: e.tensor_copy(out=gsb[:, :], in_=ps[1][:, 0:16]), reads=[("ps", 1)], writes=["gsb"])

          ck(3)
          for cg in range(8):
              pb = 4 * (cg % 2)
              for sl in range(8):
                  si, slab = load_slab(wtm[sl * 512:(sl + 1) * 512, cg * 512:(cg + 1) * 512], 4, 512)
                  for k4 in range(4):
                      kd = sl * 4 + k4
                      for tt in range(4):
                          T.op("pe", lambda e, slab=slab, k4=k4, tt=tt, kd=kd, pb=pb: e.matmul(
                              ps[pb + tt][:, :], lhsT=AT[:, kd, tt * 128:(tt + 1) * 128], rhs=slab[:, k4, :],
                              start=(kd == 0), stop=(kd == KD - 1)),
                              reads=[("slab", si), ("AT", tt, kd)], writes=[("ps", pb + tt)])
              for tt in range(4):
                  if cg < 4:
                      T.op("act" if tt % 2 else "dve",
                           (lambda e, cg=cg, tt=tt, pb=pb: e.activation(out=V[:, tt, cg * 512:(cg + 1) * 512], in_=ps[pb + tt][:, :], func=ACT.Copy)) if tt % 2 else
                           (lambda e, cg=cg, tt=tt, pb=pb: e.tensor_copy(out=V[:, tt, cg * 512:(cg + 1) * 512], in_=ps[pb + tt][:, :])),
                           reads=[("ps", pb + tt)], writes=[("V", tt)])
                  else:
                      c0 = (cg - 4) * 512
                      fn_ = ACT.Sigmoid if cg < 6 else ACT.Silu
                      tb = tmpcs[tt % 2]; tbk = ("tmpc", "tmpc2")[tt % 2]
                      T.op("act", lambda e, c0=c0, tt=tt, fn_=fn_, pb=pb, tb=tb: e.activation(out=tb[:, :], in_=ps[pb + tt][:, :], func=fn_),
                           reads=[("ps", pb + tt)], writes=[tbk])
                      T.op("dve", lambda e, c0=c0, tt=tt, tb=tb: e.tensor_tensor(out=OG[:, tt, c0:c0 + 512], in0=tb[:, :],
                                                                           in1=GN[:, c0:c0 + 512], op=ALU.mult),
                           reads=[tbk, "GN"], writes=[("OG", tt)])

          ck(4)
          for ct in range(8):
              T.op("dve", lambda e, ct=ct: e.tensor_scalar(out=tmpc[:, :], in0=pre[:, ct, 0:512], scalar1=cwb[:, ct, 0:1],
                                                           scalar2=cwb[:, ct, 4:5], op0=ALU.mult, op1=ALU.add),
                   reads=[("pre", ct), "cwb"], writes=["tmpc"])
              for j in (1, 2, 3):
                  T.op("dve", lambda e, ct=ct, j=j: e.scalar_tensor_tensor(out=tmpc[:, :], in0=pre[:, ct, j:j + 512],
                                                                           scalar=cwb[:, ct, j:j + 1], in1=tmpc[:, :],
                                                                           op0=ALU.mult, op1=ALU.add),
                       reads=[("pre", ct), "cwb", "tmpc"], writes=["tmpc"])
              T.op("act", lambda e, ct=ct: e.activation(out=qk[:, ct, :], in_=tmpc[:, :], func=ACT.Silu),
                   reads=["tmpc"], writes=[("qk", ct)])
              T.op("pool", lambda e, ct=ct: e.tensor_copy(out=pre[:, ct, 0:3], in_=pre[:, ct, 512:515]),
                   reads=[("pre", ct)], writes=[("pre", ct)])

          for hd in range(4):
              T.op("pe", lambda e, hd=hd: e.matmul(ps[4][:, :], lhsT=aw2[0:16, hd * 128:(hd + 1) * 128], rhs=gaT[0:16, :],
                                                    start=True, stop=True),
                   reads=["aw2", "gaT"], writes=[("ps", 4)])
              T.op("act", lambda e, hd=hd: e.activation(out=spt[:, :], in_=ps[4][:, :], func=ACT.Exp, scale=-1.0,
                                                         bias=nab[:, hd:hd + 1]),
                   reads=[("ps", 4), "nab"], writes=["spt"])
              T.op("act", lambda e: e.activation(out=spt[:, :], in_=spt[:, :], func=ACT.Ln, bias=1.0),
                   reads=["spt"], writes=["spt"])
              T.op("dve", lambda e: e.tensor_tensor_scan(out=csp[:, :], data0=segm[:, :], data1=spt[:, :], initial=0.0,
                                                         op0=ALU.mult, op1=ALU.add),
                   reads=["spt", "segm"], writes=["csp"])
              T.op("act", lambda e, hd=hd: e.activation(out=EQg[:, hd, :], in_=csp[:, :], func=ACT.Exp, scale=-1.0 / 16.0),
                   reads=["csp"], writes=[("EQg", hd)])
              T.op("act", lambda e, hd=hd: e.activation(out=EKg[:, hd, :], in_=csp[:, :], func=ACT.Exp, scale=1.0 / 16.0),
                   reads=["csp"], writes=[("EKg", hd)])

          ck(5)
          flush_cc()
          for tt in range(4):
              c = it * 4 + tt
              tsl = slice(tt * 128, (tt + 1) * 128)
              mt = mixtok[c % 2]
              mtk = f"mixtok{c % 2}"
              for h in range(2):
                  T.op("act", lambda e, h=h, tt=tt: e.activation(out=nls[:, h:h + 1], in_=gsb[:, tt * 4 + 2 + h:tt * 4 + 3 + h],
                                                                 func=ACT.Exp, scale=-1.0, bias=ngb[:, 2 + h:3 + h]),
                       reads=["gsb", "ngb"], writes=["nls"])
              T.op("act", lambda e: e.activation(out=nls[:, :], in_=nls[:, :], func=ACT.Ln, bias=1.0),
                   reads=["nls"], writes=["nls"])
              T.op("dve", lambda e, tt=tt: e.tensor_tensor(out=ival[:, :], in0=gsb[:, tt * 4:tt * 4 + 2], in1=gb[:, 0:2], op=ALU.add),
                   reads=["gsb", "gb"], writes=["ival"])
              for h in range(2):
                  T.op("dve", lambda e, h=h: e.tensor_scalar(out=rb[:, :], in0=tri[:, :], scalar1=nls[:, h:h + 1], scalar2=None,
                                                             op0=ALU.mult), reads=["tri", "nls"], writes=["rb"])
                  T.op("dve", lambda e, h=h: e.scalar_tensor_tensor(out=ru[:, :], in0=ident_f[:, :], scalar=ival[:, h:h + 1],
                                                                    in1=rb[:, :], op0=ALU.mult, op1=ALU.add),
                       reads=["ident_f", "ival", "rb"], writes=["ru"])
                  T.op("pe", lambda e: e.matmul(ps[4][:, 0:128], lhsT=ones_f[:, :], rhs=rb[:, :], start=True, stop=True),
                       reads=["ones_f", "rb"], writes=[("ps", 4)])
                  T.op("pe", lambda e: e.matmul(ps[4][:, 128:256], lhsT=ones_f[:, :], rhs=ru[:, :], start=True, stop=True),
                       reads=["ones_f", "ru"], writes=[("ps", 4)])
                  T.op("act", lambda e, h=h: e.activation(out=EQm[:, h, :], in_=ps[4][:, 0:128], func=ACT.Exp, scale=-1.0),
                       reads=[("ps", 4)], writes=[("EQm", h)])
                  T.op("act", lambda e, h=h: e.activation(out=EKm[:, h, :], in_=ps[4][:, 128:256], func=ACT.Exp),
                       reads=[("ps", 4)], writes=[("EKm", h)])

              def stage(hi, st, tt=tt, mt=mt, mtk=mtk, tsl=tsl):
                  ism = hi < 2
                  h = hi % 2
                  s_ = hi % 2
                  bA, bN = ((5, 6), (7, 3))[s_]
                  qsk, ksk, atk, ktk = ("qs", s_), ("ks", s_), ("ATm", s_), ("ktok", s_)
                  smk = f"smo{s_}"
                  c0 = 16 + 4 * s_
                  vsl = slice(hi * 512, (hi + 1) * 512)
                  dcol = 300 + 2 * hi
                  if st == 1:
                      for dt in range(2):
                          if ism:
                              qsrc = qk[:, 2 * h + dt, tsl]; ksrc = qk[:, 4 + 2 * h + dt, tsl]
                              eq = EQm[:, h, :]; ek = EKm[:, h, :]
                              rq = [("qk", 2 * h + dt), ("EQm", h)]; rk = [("qk", 4 + 2 * h + dt), ("EKm", h)]
                          else:
                              qsrc = gqk[:, 2 * h + dt, tsl]; ksrc = gqk[:, 4 + 2 * h + dt, tsl]
                              eq = EQg[:, 2 * h + dt, tsl]; ek = EKg[:, 2 * h + dt, tsl]
                              rq = [("gqk", 2 * h + dt), ("EQg", 2 * h + dt)]; rk = [("gqk", 4 + 2 * h + dt), ("EKg", 2 * h + dt)]
                          T.op("dve", lambda e, qsrc=qsrc, eq=eq, dt=dt: e.scalar_tensor_tensor(
                              out=qs[s_][:, dt, :], in0=qsrc, scalar=0.0625, in1=eq, op0=ALU.mult, op1=ALU.mult),
                              reads=rq, writes=[qsk])
                          T.op("pool", lambda e, ksrc=ksrc, ek=ek, dt=dt: e.tensor_tensor(
                              out=ks[s_][:, dt, :], in0=ksrc, in1=ek, op=ALU.mult), reads=rk, writes=[ksk])
                  elif st == 2:
                      for dt in range(2):
                          T.op("pe", lambda e, dt=dt: e.matmul(ps[bA][:, 0:128], lhsT=ks[s_][:, dt, :], rhs=qs[s_][:, dt, :],
                                                                start=(dt == 0), stop=(dt == 1)),
                               reads=[ksk, qsk], writes=[("ps", bA)])
                      for dt in range(2):
                          T.op("pe", lambda e, dt=dt: e.matmul(ps[bA][:, 128 + dt * 128:256 + dt * 128], lhsT=ks[s_][:, dt, :],
                                                                rhs=ident_b[:, :], start=True, stop=True),
                               reads=[ksk, "ident_b"], writes=[("ps", bA)])
                  elif st == 3:
                      T.op("dve", lambda e: e.tensor_tensor(out=ATm[s_][:, :], in0=ps[bA][:, 0:128], in1=tri[:, :], op=ALU.mult),
                           reads=[("ps", bA), "tri"], writes=[atk])
                      T.op("act", lambda e: e.activation(out=ktok[s_][:, :], in_=ps[bA][:, 128:384], func=ACT.Copy),
                           reads=[("ps", bA)], writes=[ktk])
                  elif st == 4:
                      T.op("pe", lambda e: e.matmul(ps[bN][:, :], lhsT=ATm[s_][:, :], rhs=V[:, tt, vsl], start=True, stop=False),
                           reads=[atk, ("V", tt)], writes=[("ps", bN)])
                      for dt in range(2):
                          T.op("pe", lambda e, dt=dt: e.matmul(ps[bN][:, :], lhsT=qs[s_][:, dt, :], rhs=Sbf[:, hi * 2 + dt, :],
                                                                start=False, stop=(dt == 1)),
                               reads=[qsk, ("Sbf", hi * 2 + dt)], writes=[("ps", bN)])
                      if ism:
                          T.op("pe", lambda e: e.matmul(ps[4][:, dcol:dcol + 2], lhsT=ATm[s_][:, :], rhs=ones_b[:, :], start=True, stop=False),
                               reads=[atk, "ones_b"], writes=[("ps", 4)])
                          for dt in range(2):
                              T.op("pe", lambda e, dt=dt: e.matmul(ps[4][:, dcol:dcol + 2], lhsT=qs[s_][:, dt, :], rhs=nbf[:, h * 2 + dt, :],
                                                                    start=False, stop=(dt == 1)),
                                   reads=[qsk, ("nbf", h * 2 + dt)], writes=[("ps", 4)])
                  elif st == 5:
                      T.op("act", lambda e: e.activation(out=junk[:, 0:512], in_=ps[bN][:, :], func=ACT.Square, accum_out=sm[:, c0 + 1:c0 + 2]),
                           reads=[("ps", bN)], writes=["junk", smk])
                      a1 = sm[:, c0 + 1:c0 + 2]; a2 = sm[:, c0 + 2:c0 + 3]; a3 = sm[:, c0 + 3:c0 + 4]
                      if ism:
                          T.op("dve", lambda e: e.tensor_copy(out=a3, in_=ps[4][:, dcol:dcol + 1]), reads=[("ps", 4)], writes=[smk])
                          T.op("dve", lambda e: e.tensor_tensor(out=a2, in0=a3, in1=a3, op=ALU.mult), reads=[smk], writes=[smk])
                          T.op("dve", lambda e: e.tensor_scalar(out=a2, in0=a2, scalar1=1.0, scalar2=None, op0=ALU.max), reads=[smk], writes=[smk])
                          T.op("dve", lambda e: e.reciprocal(out=a2, in_=a2), reads=[smk], writes=[smk])
                          T.op("dve", lambda e: e.tensor_tensor(out=a1, in0=a1, in1=a2, op=ALU.mult), reads=[smk], writes=[smk])
                          T.op("dve", lambda e: e.tensor_scalar(out=a1, in0=a1, scalar1=1.0 / 512, scalar2=EPS, op0=ALU.mult, op1=ALU.add),
                               reads=[smk], writes=[smk])
                          T.op("dve", lambda e: e.reciprocal(out=a1, in_=a1), reads=[smk], writes=[smk])
                          T.op("dve", lambda e: e.tensor_tensor(out=a1, in0=a1, in1=a2, op=ALU.mult), reads=[smk], writes=[smk])
                          T.op("act", lambda e: e.activation(out=a1, in_=a1, func=ACT.Sqrt), reads=[smk], writes=[smk])
                      else:
                          rstd_from_ssq(a1, a1, 512, smk)
                      T.op("dve", lambda e: e.scalar_tensor_tensor(
                          out=mt[:, vsl], in0=ps[bN][:, :], scalar=a1, in1=OG[:, tt, vsl], op0=ALU.mult, op1=ALU.mult),
                          reads=[("ps", bN), smk, ("OG", tt)], writes=[mtk])
                  elif st == 6:
                      for dt in range(2):
                          si_ = hi * 2 + dt
                          if ism:
                              eL = EQm[:, h, 127:128]; reL = [("EQm", h)]
                          else:
                              eL = EQg[:, 2 * h + dt, tt * 128 + 127:tt * 128 + 128]; reL = [("EQg", 2 * h + dt)]
                          T.op("pe", lambda e, dt=dt: e.matmul(ps[bA][:, :], lhsT=ktok[s_][:, dt * 128:(dt + 1) * 128],
                                                                rhs=V[:, tt, vsl], start=True, stop=True),
                               reads=[ktk, ("V", tt)], writes=[("ps", bA)])
                          T.op("dve", lambda e, si_=si_: e.tensor_tensor(out=S32[:, si_, :], in0=ps[bA][:, :], in1=S32[:, si_, :], op=ALU.add),
                               reads=[("ps", bA), ("S32", si_)], writes=[("S32", si_)])
                          T.op("act", lambda e, si_=si_, eL=eL: e.activation(out=S32[:, si_, :], in_=S32[:, si_, :], func=ACT.Copy, scale=eL),
                               reads=[("S32", si_)] + reL, writes=[("S32", si_)])
                          T.op("pool", lambda e, si_=si_: e.tensor_copy(out=Sbf[:, si_, :], in_=S32[:, si_, :]),
                               reads=[("S32", si_)], writes=[("Sbf", si_)])
                          if ism:
                              ni = h * 2 + dt
                              ncol = 320 + 2 * ni
                              T.op("pe", lambda e, dt=dt, ncol=ncol: e.matmul(ps[4][:, ncol:ncol + 2], lhsT=ktok[s_][:, dt * 128:(dt + 1) * 128],
                                                                               rhs=ones_b[:, :], start=True, stop=True),
                                   reads=[ktk, "ones_b"], writes=[("ps", 4)])
                              T.op("dve", lambda e, ni=ni, ncol=ncol: e.tensor_tensor(out=n32[:, ni, :], in0=ps[4][:, ncol:ncol + 2], in1=n32[:, ni, :], op=ALU.add),
                                   reads=[("ps", 4), ("n32", ni)], writes=[("n32", ni)])
                              T.op("dve", lambda e, ni=ni, eL=eL: e.tensor_scalar(out=n32[:, ni, :], in0=n32[:, ni, :], scalar1=eL, scalar2=None,
                                                                                  op0=ALU.mult),
                                   reads=[("n32", ni)] + reL, writes=[("n32", ni)])
                              T.op("dve", lambda e, ni=ni: e.tensor_copy(out=nbf[:, ni, :], in_=n32[:, ni, :]),
                                   reads=[("n32", ni)], writes=[("nbf", ni)])

              gen = emit_xT(it + 1, tt) if it + 1 < 8 else iter(())
              for pair in ((0, 1), (2, 3)):
                  for st in range(1, 7):
                      for hi in pair:
                          stage(hi, st)
                      next(gen, None)
              for _ in gen:
                  pass
              ck(6)
              if c <= 15:
                  T.dma("sync", send[128 + 128 * c:256 + 128 * c, :], mt[:], reads=[mtk], writes=[("send", 128 + 128 * c)])
              if c >= 15:
                  T.dma("sync", send[SH + 128 * (c - 15):SH + 128 * (c - 14), :], mt[:], reads=[mtk],
                        writes=[("send", SH + 128 * (c - 15))])
              if c % 2 == 0 and c <= 14:
                  pending.append(c // 2)
              elif c == 15:
                  pending.append(8)
              elif c % 2 == 1 and c >= 17:
                  pending.append((c + 1) // 2)

          ck(7)
      ck(8)
    except StopEmit:
        stopped = True
        if os.environ.get("KCC"):
            stopped = False
            pending[:] = list(range(17))
    if not stopped:
        flush_cc()
    if KDBG:
        for p_ in range(17):
            T.dma("sync", dbg_send[p_ * 256:(p_ + 1) * 256, :], send[p_ * 256:(p_ + 1) * 256, :],
                  reads=[("send", p_ * 256), ("send", p_ * 256 + 128)], writes=["dbg_send"])
    T.barrier()
    phase1_lists = T.lists
    T.lists = {k: [] for k in phase1_lists}

    def replay(e, lst):
        for waits, fn, inc in lst:
            for k, v in waits:
                e.wait_ge(sems[k], v)
            if fn is not None:
                ins = fn(e)
                ins.then_inc(sems[inc[0]], inc[1])

    def run_block(lists):
        with nc.Block() as block:
            @block.tensor
            def _(e):
                replay(e, lists["pe"])

            @block.scalar
            def _(e):
                replay(e, lists["act"])

            @block.vector
            def _(e):
                replay(e, lists["dve"])

            @block.gpsimd
            def _(e):
                replay(e, lists["pool"])

            @block.sync
            def _(e):
                replay(e, lists["sync"])

    run_block(phase1_lists)
    p1.close()

    x1buf = sb("x1buf", [P, 4, D]); G3 = sb("G3", [P, D])
    slabs.append(sb("slab3", [P, 2048], BF16))
    slabs.append(sb("slab4", [P, 2048], BF16))
    slabs.append(sb("slab5", [P, 2048], BF16))
    act_sb = sb("act_sb", [P, 44, 512], BF16)
    cand = [sb(f"cand{i}", [P, 1024], BF16) for i in range(2)]
    mixrow = sb("mixrow", [P, D], BF16)
    gbuf = sb("gbuf", [P, 514]); gc = sb("gc", [P, 512])
    ghalo = sb("ghalo", [P, NF, 2]); rbc = sb("rbc", [P, P]); dgm = sb("dgm", [P, P])

    T.dma("sync", G3[:], g3_d.partition_broadcast(P), writes=["G3"])
    set_i = [0]

    def next_set():
        s_ = set_i[0] % 2
        set_i[0] += 1
        return s_

    try:
        if stopped or KSTOP == 9 or os.environ.get("KCC") == "1":
            raise StopEmit()
        def emit_mix_prep(r0_, tt, bb):
            r = r0_ + tt * 128
            for rk in range(2):
                for hf in range(2):
                    c0_ = rk * 2048 + hf * 1024
                    for s_ in range(2):
                        gi_ = s_ * SH + r
                        gr_ = (gi_ // 256) * 512 + rk * 256 + (gi_ % 256)
                        T.dma("sync", cand[s_][:], gath[gr_:gr_ + 128, hf * 1024:(hf + 1) * 1024],
                              reads=[("gath", gi_ // 256)], writes=[f"cand{s_}"])
                    T.op("dve", lambda e, c0_=c0_: e.tensor_scalar(out=mixrow[:, c0_:c0_ + 1024], in0=cand[0][:], scalar1=sel[:, 0:1],
                                                                   scalar2=None, op0=ALU.mult),
                         reads=["cand0", "sel"], writes=["mixrow"])
                    T.op("dve", lambda e, c0_=c0_: e.scalar_tensor_tensor(out=mixrow[:, c0_:c0_ + 1024], in0=cand[1][:], scalar=sel[:, 1:2],
                                                                          in1=mixrow[:, c0_:c0_ + 1024], op0=ALU.mult, op1=ALU.add),
                         reads=["cand1", "sel", "mixrow"], writes=["mixrow"])
            for q in range(8):
                bank = bb + (q % 2)
                for j in range(4):
                    kc = 4 * q + j
                    T.op("pe", lambda e, kc=kc, j=j, bank=bank: e.matmul(ps[bank][:, j * 128:(j + 1) * 128],
                                                                          lhsT=mixrow[:, kc * 128:(kc + 1) * 128], rhs=ident_b[:, :],
                                                                          start=True, stop=True),
                         reads=["mixrow", "ident_b"], writes=[("ps", bank)])
                for j in range(4):
                    kc = 4 * q + j
                    T.op("act" if bank % 2 else "dve",
                         (lambda e, kc=kc, j=j, bank=bank, tt=tt: e.activation(out=AT[:, kc, tt * 128:(tt + 1) * 128],
                                                                                 in_=ps[bank][:, j * 128:(j + 1) * 128], func=ACT.Copy)) if bank % 2 else
                         (lambda e, kc=kc, j=j, bank=bank, tt=tt: e.tensor_copy(out=AT[:, kc, tt * 128:(tt + 1) * 128],
                                                                                  in_=ps[bank][:, j * 128:(j + 1) * 128])),
                         reads=[("ps", bank)], writes=[("AT", tt)])

        halves = [(0, 44), (44, 42)]
        for tile in range(-1, 4):
            ntt = 1 if tile < 0 else 4
            Tn = ntt * 128
            r0 = 0 if tile < 0 else 128 + 512 * tile
            for tt in range(ntt):
                r = r0 + tt * 128
                T.dma("sync", x1buf[:, tt, :], x2[r:r + 128, :], writes=[("x1buf", tt)])
                if tile <= 0:
                    emit_mix_prep(r0, tt, 0)
            ATt = [("AT", t) for t in range(ntt)]
            for dg in range(8):
                s_ = next_set()
                for sl in range(8):
                    si, slab = load_slab(wo[sl * 512:(sl + 1) * 512, dg * 512:(dg + 1) * 512], 4, 512)
                    for k4 in range(4):
                        kc = sl * 4 + k4
                        for tt in range(ntt):
                            T.op("pe", lambda e, slab=slab, k4=k4, tt=tt, kc=kc, s_=s_: e.matmul(
                                ps[s_ * 4 + tt][:, :], lhsT=AT[:, kc, tt * 128:(tt + 1) * 128], rhs=slab[:, k4, :],
                                start=(kc == 0), stop=(kc == KD - 1)),
                                reads=[("slab", si), ("AT", tt)], writes=[("ps", s_ * 4 + tt)])
                for tt in range(ntt):
                    T.op("dve", lambda e, tt=tt, dg=dg, s_=s_: e.tensor_tensor(
                        out=x1buf[:, tt, dg * 512:(dg + 1) * 512], in0=ps[s_ * 4 + tt][:, :], in1=x1buf[:, tt, dg * 512:(dg + 1) * 512], op=ALU.add),
                        reads=[("ps", s_ * 4 + tt), ("x1buf", tt)], writes=[("x1buf", tt)])
            if KDBG and tile >= 0:
                for tt in range(4):
                    T.dma("sync", dbg_x1[tile * 512 + tt * 128:tile * 512 + tt * 128 + 128, :], x1buf[:, tt, :],
                          reads=[("x1buf", tt)], writes=["dbg_x1"])
            for tt in range(ntt):
                T.op("act", lambda e, tt=tt: e.activation(out=mixrow[:, :], in_=x1buf[:, tt, :], func=ACT.Square, accum_out=sm[:, 4:5]),
                     reads=[("x1buf", tt)], writes=["mixrow", "sm4"])
                rstd_from_ssq(sm[:, 4:5], sm[:, 4:5], D, "sm4")
                T.op("dve", lambda e: e.tensor_scalar(out=dgm[:, :], in0=ident_f[:, :], scalar1=sm[:, 4:5], scalar2=None, op0=ALU.mult),
                     reads=["ident_f", "sm4"], writes=["dgm"])
                T.op("pe", lambda e: e.matmul(ps[7][:, 0:128], lhsT=ones_f[:, :], rhs=dgm[:, :], start=True, stop=True),
                     reads=["ones_f", "dgm"], writes=[("ps", 7)])
                T.op("act", lambda e: e.activation(out=rbc[:, :], in_=ps[7][:, 0:128], func=ACT.Copy), reads=[("ps", 7)], writes=["rbc"])
                for q in range(8):
                    bank = 4 + (q % 2)
                    for j in range(4):
                        kd = 4 * q + j
                        T.op("pe", lambda e, kd=kd, j=j, bank=bank, tt=tt: e.transpose(
                            out=ps[bank][:, j * 128:(j + 1) * 128], in_=x1buf[:, tt, kd * 128:(kd + 1) * 128], identity=ident_f[:]),
                            reads=[("x1buf", tt), "ident_f"], writes=[("ps", bank)])
                    for j in range(4):
                        kd = 4 * q + j
                        T.op("dve", lambda e, kd=kd, j=j, bank=bank, tt=tt: e.scalar_tensor_tensor(
                            out=AT[:, kd, tt * 128:(tt + 1) * 128], in0=ps[bank][:, j * 128:(j + 1) * 128], scalar=g2t[:, kd:kd + 1],
                            in1=rbc[:, :], op0=ALU.mult, op1=ALU.mult),
                            reads=[("ps", bank), "g2t", "rbc"], writes=[("AT", tt)])
            if tile < 0:
                for fa in range(0, NF, 4):
                    nfg = min(4, NF - fa)
                    s_ = next_set()
                    for sl in range(8):
                        si, slab = load_slab(wg[sl * 512:(sl + 1) * 512, fa * 128:(fa + nfg) * 128], 4, nfg * 128)
                        for k4 in range(4):
                            kd = sl * 4 + k4
                            for u in range(nfg):
                                bk = s_ * 4 + u
                                T.op("pe", lambda e, slab=slab, k4=k4, u=u, kd=kd, bk=bk: e.matmul(
                                    ps[bk][:, 0:128], lhsT=slab[:, k4, u * 128:(u + 1) * 128], rhs=AT[:, kd, 0:128],
                                    start=(kd == 0), stop=(kd == KD - 1)),
                                    reads=[("slab", si)] + ATt, writes=[("ps", bk)])
                    for u in range(nfg):
                        bk = s_ * 4 + u
                        T.op("dve" if u % 2 else "act",
                             (lambda e, f=fa + u, bk=bk: e.tensor_copy(out=ghalo[:, f, :], in_=ps[bk][:, 126:128])) if u % 2 else
                             (lambda e, f=fa + u, bk=bk: e.activation(out=ghalo[:, f, :], in_=ps[bk][:, 126:128], func=ACT.Copy)),
                             reads=[("ps", bk)], writes=[("ghalo", fa + u)])
                ck(10)
                continue
            for (f0, nfh) in halves:
                for fa in range(f0, f0 + nfh, 2):
                    s_ = next_set()
                    for wi, W in enumerate((wg, wu)):
                        if tile < 0 and wi == 1:
                            continue
                        for sl in range(4):
                            si, slab = load_slab(W[sl * 1024:(sl + 1) * 1024, fa * 128:(fa + 2) * 128], 8, 256)
                            for k8 in range(8):
                                kd = sl * 8 + k8
                                for u in range(2):
                                    bk = s_ * 4 + wi * 2 + u
                                    T.op("pe", lambda e, slab=slab, k8=k8, u=u, kd=kd, bk=bk, Tn=Tn: e.matmul(
                                        ps[bk][:, 0:Tn], lhsT=slab[:, k8, u * 128:(u + 1) * 128], rhs=AT[:, kd, 0:Tn],
                                        start=(kd == 0), stop=(kd == KD - 1)),
                                        reads=[("slab", si)] + ATt, writes=[("ps", bk)])
                    for u in range(2):
                        f = fa + u
                        bg = s_ * 4 + u
                        bu = s_ * 4 + 2 + u
                        if tile < 0:
                            T.op("dve", lambda e, f=f, bg=bg, Tn=Tn: e.tensor_copy(out=ghalo[:, f, :], in_=ps[bg][:, Tn - 2:Tn]),
                                 reads=[("ps", bg)], writes=["ghalo"])
                            continue
                        T.op("act", lambda e, bg=bg: e.activation(out=gbuf[:, 2:514], in_=ps[bg][:, :], func=ACT.Copy),
                             reads=[("ps", bg)], writes=["gbuf"])
                        T.op("pool", lambda e, f=f: e.tensor_copy(out=gbuf[:, 0:2], in_=ghalo[:, f, :]), reads=[("ghalo", f)], writes=["gbuf"])
                        T.op("dve", lambda e, f=f: e.tensor_scalar(out=gc[:, :], in0=gbuf[:, 0:512], scalar1=fcw[:, f, 0:1], scalar2=fcw[:, f, 3:4],
                                                                   op0=ALU.mult, op1=ALU.add), reads=["gbuf", "fcw"], writes=["gc"])
                        for j in (1, 2):
                            T.op("dve", lambda e, f=f, j=j: e.scalar_tensor_tensor(out=gc[:, :], in0=gbuf[:, j:j + 512], scalar=fcw[:, f, j:j + 1],
                                                                                   in1=gc[:, :], op0=ALU.mult, op1=ALU.add),
                                 reads=["gbuf", "fcw", "gc"], writes=["gc"])
                        T.op("pool", lambda e, f=f: e.tensor_copy(out=ghalo[:, f, :], in_=gbuf[:, 512:514]), reads=["gbuf"], writes=[("ghalo", f)])
                        T.op("act", lambda e: e.activation(out=gc[:, :], in_=gc[:, :], func=ACT.Silu), reads=["gc"], writes=["gc"])
                        T.op("dve", lambda e, f=f, f0=f0, bu=bu: e.tensor_tensor(out=act_sb[:, f - f0, :], in0=gc[:, :], in1=ps[bu][:, :], op=ALU.mult),
                             reads=["gc", ("ps", bu)], writes=[("act", f - f0)])
                if tile < 0:
                    continue
                for dg in range(8):
                    s_ = next_set()
                    for fl0 in range(0, nfh, 4):
                        nk = min(4, nfh - fl0)
                        si, slab = load_slab(wd[(f0 + fl0) * 128:(f0 + fl0 + nk) * 128, dg * 512:(dg + 1) * 512], nk, 512)
                        for k4 in range(nk):
                            fl = fl0 + k4
                            for tt in range(4):
                                T.op("pe", lambda e, slab=slab, k4=k4, tt=tt, fl=fl, s_=s_, nfh=nfh: e.matmul(
                                    ps[s_ * 4 + tt][:, :], lhsT=act_sb[:, fl, tt * 128:(tt + 1) * 128], rhs=slab[:, k4, :],
                                    start=(fl == 0), stop=(fl == nfh - 1)),
                                    reads=[("slab", si), ("act", fl)], writes=[("ps", s_ * 4 + tt)])
                    if f0 > 0 and tile < 3 and dg % 2 == 1:
                        emit_mix_prep(128 + 512 * (tile + 1), (dg - 1) // 2, 4 * (1 - s_))
                    for tt in range(4):
                        T.op("dve", lambda e, tt=tt, dg=dg, s_=s_: e.tensor_tensor(
                            out=x1buf[:, tt, dg * 512:(dg + 1) * 512], in0=ps[s_ * 4 + tt][:, :], in1=x1buf[:, tt, dg * 512:(dg + 1) * 512], op=ALU.add),
                            reads=[("ps", s_ * 4 + tt), ("x1buf", tt)], writes=[("x1buf", tt)])
            if tile < 0:
                ck(10)
                continue
            for tt in range(4):
                T.op("act", lambda e, tt=tt: e.activation(out=mixrow[:, :], in_=x1buf[:, tt, :], func=ACT.Square, accum_out=sm[:, 5:6]),
                     reads=[("x1buf", tt)], writes=["mixrow", "sm5"])
                rstd_from_ssq(sm[:, 5:6], sm[:, 5:6], D, "sm5")
                T.op("dve", lambda e, tt=tt: e.scalar_tensor_tensor(out=x1buf[:, tt, :], in0=x1buf[:, tt, :], scalar=sm[:, 5:6], in1=G3[:, :],
                                                                    op0=ALU.mult, op1=ALU.mult),
                     reads=[("x1buf", tt), "sm5", "G3"], writes=[("x1buf", tt)])
                ro = tile * 512 + tt * 128
                T.dma("sync", out[ro:ro + 128, :], x1buf[:, tt, :], reads=[("x1buf", tt)], writes=["out"])

    except StopEmit:
        pass
    T.barrier()
    run_block(T.lists)
    es.close()
    return nc


_NC = None


def _prep_inputs(inp):
    f32 = np.float32
    g = lambda k: np.asarray(inp[k], dtype=f32)
    x = g("x"); w_in = g("w_in"); w_out = g("w_out")
    offs = np.cumsum([0, 1024, 1024, 2048, 2048, 4, 4, 1024, 1024, 2048, 2048, 16])
    o_mq, o_mk, o_mv, o_mo, o_mi, o_mf, o_gq, o_gk, o_gv, o_gg, o_ga = offs[:11]
    conv_w = g("mlstm_conv_w"); conv_b = g("mlstm_conv_b")
    mnorm = g("mlstm_norm_g"); gnorm = g("gla_norm_g")
    aw2 = g("gla_a_w2"); a_b = g("gla_a_b")
    i_b = g("mlstm_i_b"); f_b = g("mlstm_f_b")
    ident = np.eye(P, dtype=f32)
    tri = np.triu(np.ones((P, P), f32))
    segm = np.ones((P, 512), f32); segm[:, ::128] = 0.0
    wg = np.ascontiguousarray(g("w_ffn_gate")); wu = np.ascontiguousarray(g("w_ffn_up")); wd = np.ascontiguousarray(g("w_ffn_down"))
    fc = np.concatenate([g("ffn_conv_w"), g("ffn_conv_b")[None, :]], 0)
    fcw = np.ascontiguousarray(fc.T.reshape(NF, P, 4).transpose(1, 0, 2))
    tk = lambda v: np.ascontiguousarray(v.reshape(KD, P).T)
    g1t = tk(g("ln1_g")); g2t = tk(g("ln2_g")); g3 = g("lnf_g")[None, :].copy()
    rows = []
    for r in range(2):
        for h in (2 * r, 2 * r + 1):
            rows.append(np.arange(h * 512, (h + 1) * 512))
        for h in (2 * r, 2 * r + 1):
            rows.append(2048 + np.arange(h * 512, (h + 1) * 512))
    wo = np.ascontiguousarray(w_out[np.concatenate(rows)])
    maps = []
    for core in range(8):
        b, gi = core // 2, core % 2
        hs = (2 * gi, 2 * gi + 1)
        c256 = lambda base: np.concatenate([np.arange(base + h * 256, base + (h + 1) * 256) for h in hs])
        c512 = lambda base: np.concatenate([np.arange(base + h * 512, base + (h + 1) * 512) for h in hs])
        wfm = np.ascontiguousarray(w_in[:, np.concatenate([c256(o_mq), c256(o_mk), c256(o_gq), c256(o_gk)])])
        wtm = np.ascontiguousarray(w_in[:, np.concatenate([c512(o_mv), c512(o_gv), c512(o_mo), c512(o_gg)])])
        wsm = np.ascontiguousarray(w_in[:, np.concatenate([np.arange(o_ga, o_ga + 16), o_mi + np.array(hs), o_mf + np.array(hs)])])
        x2 = np.zeros((SH, D), f32)
        lo = 2048 * gi - 128
        if lo < 0:
            x2[128:] = x[b, 0:2048]
        else:
            x2[:] = x[b, lo:lo + SH]
        ccols = np.concatenate([c256(0), c256(1024)])
        cw = np.concatenate([conv_w[:, ccols], conv_b[None, ccols]], 0)
        cwb = np.ascontiguousarray(cw.T.reshape(8, P, 5).transpose(1, 0, 2))
        gbv = np.array([i_b[hs[0]], i_b[hs[1]], f_b[hs[0]], f_b[hs[1]]], f32)
        gb = np.ascontiguousarray(np.tile(gbv[None, :], (P, 1)))
        acols = c256(0)
        ab = np.ascontiguousarray(a_b[acols].reshape(4, P).T)
        gn = np.concatenate([mnorm[c512(0)], gnorm[c512(0)]])[None, :].copy()
        selv = np.zeros((P, 2), f32); selv[:, gi] = 1.0
        maps.append(dict(x1=np.ascontiguousarray(x[b]), x2=x2, wfm=wfm, wtm=wtm, wsm=wsm, wo=wo, wg=wg, wu=wu, wd=wd,
                         g1t=g1t, g2t=g2t, g3=g3, gn=gn, cwb=cwb, gb=gb, aw2=np.ascontiguousarray(aw2[:, acols]), ab=ab,
                         fcw=fcw, sel=selv, ident=ident, tri=tri, segm=segm))
    return maps


def kernel(**inputs):
    global _NC
    if _NC is None:
        _NC = build_nc()
    maps = _prep_inputs(inputs)
    res = run_bass_kernel_spmd(_NC, maps, core_ids=list(range(8)))
    outs = [np.asarray(res.results[c]["out"], dtype=np.float32) for c in range(8)]
    full = np.stack([np.concatenate([outs[2 * b], outs[2 * b + 1]], 0) for b in range(4)], 0)
    return full
```
